# Optimizing a Trainium2 kernel written in Bass

```python
import math
import jax, jax.numpy as jnp
from jax import lax
import numpy as np

D_MODEL = 1024
BATCH = 32
SEQ = 2048
DEPTH = 1

D_MIX = D_MODEL
D_S5 = D_MIX // 2
D_ML = D_MIX - D_S5
S5_GROUP = 16
S5_NG = D_S5 // S5_GROUP
S5_P = 64
ML_HEADS = 4
ML_DH = D_ML // ML_HEADS
ML_CHUNK = 128
CONV_K = 5
N_GATES = 4
D_IN = D_S5 + 2 * D_ML + N_GATES * ML_HEADS
D_FF = 256 * math.ceil(8 * D_MODEL / 3 / 256)
EPS = 1e-6

kernel_name = "hybrid_s5_mlstm_bidir_block"


def rmsnorm(x, g):
    x32 = x.astype(jnp.float32)
    y = x32 * lax.rsqrt(jnp.mean(x32 * x32, axis=-1, keepdims=True) + EPS)
    return (y * g.astype(jnp.float32)).astype(x.dtype)


def s5_discretise(lam_re, lam_im, log_dt, b_re, b_im):
    f32 = jnp.float32
    lr, li = lam_re.astype(f32), lam_im.astype(f32)
    dt = jnp.exp(log_dt.astype(f32))[:, None]
    mag = jnp.exp(lr * dt)
    bar_re, bar_im = mag * jnp.cos(li * dt), mag * jnp.sin(li * dt)
    xr, xi = bar_re - 1.0, bar_im
    den = lr * lr + li * li
    fr = (xr * lr + xi * li) / den
    fi = (xi * lr - xr * li) / den
    br, bi = b_re.astype(f32), b_im.astype(f32)
    bb_re = fr[..., None] * br - fi[..., None] * bi
    bb_im = fr[..., None] * bi + fi[..., None] * br
    return bar_re, bar_im, bb_re, bb_im


def _linear_recurrence_combine(e1, e2):
    a1r, a1i, b1r, b1i = e1
    a2r, a2i, b2r, b2i = e2
    return (a2r * a1r - a2i * a1i,
            a2r * a1i + a2i * a1r,
            a2r * b1r - a2i * b1i + b2r,
            a2r * b1i + a2i * b1r + b2i)


def s5_scan(u, lam_re, lam_im, log_dt, b_re, b_im, reverse):
    bar_re, bar_im, bb_re, bb_im = s5_discretise(lam_re, lam_im, log_dt, b_re, b_im)
    bu_re = jnp.einsum('bsgc,gpc->bsgp', u, bb_re)
    bu_im = jnp.einsum('bsgc,gpc->bsgp', u, bb_im)
    a_re = jnp.broadcast_to(bar_re, bu_re.shape)
    a_im = jnp.broadcast_to(bar_im, bu_im.shape)
    _, _, s_re, s_im = lax.associative_scan(
        _linear_recurrence_combine, (a_re, a_im, bu_re, bu_im), axis=1, reverse=reverse)
    return s_re, s_im


def s5_group(u, lam_re, lam_im, log_dt, b_re, b_im, c_re, c_im, d, w_glu, b_glu):
    bsz, seq, _ = u.shape
    f32 = jnp.float32
    u32 = u.astype(f32)
    ug = u32.reshape(bsz, seq, S5_NG, S5_GROUP)
    sf_re, sf_im = s5_scan(ug, lam_re[0], lam_im[0], log_dt[0], b_re, b_im, False)
    sb_re, sb_im = s5_scan(ug, lam_re[1], lam_im[1], log_dt[1], b_re, b_im, True)
    s_re, s_im = sf_re + sb_re, sf_im + sb_im
    y = (jnp.einsum('bsgp,gcp->bsgc', s_re, c_re.astype(f32))
         - jnp.einsum('bsgp,gcp->bsgc', s_im, c_im.astype(f32)))
    y = y.reshape(bsz, seq, D_S5) + d.astype(f32) * u32
    y = jax.nn.gelu(y)
    out = y * jax.nn.sigmoid(y @ w_glu.astype(f32) + b_glu.astype(f32))
    return out.astype(u.dtype)


def _mlstm_chunk_step(carry, inp):
    c_state, n_state, m_state = carry
    q, k, v, li, lf = inp
    L = q.shape[2]
    b = jnp.cumsum(lf, axis=-1)
    a = b + m_state[..., None]
    lower_tri = jnp.tril(jnp.ones((L, L), dtype=bool))
    d = jnp.where(lower_tri, b[..., :, None] - b[..., None, :] + li[..., None, :], -jnp.inf)
    m_t = jnp.maximum(a, jnp.max(d, axis=-1))
    w_intra = jnp.exp(d - m_t[..., None])
    w_inter = jnp.exp(a - m_t)
    s = jnp.einsum('bhtd,bhsd->bhts', q, k) * w_intra
    num = (jnp.einsum('bhts,bhsd->bhtd', s, v)
           + w_inter[..., None] * jnp.einsum('bhvk,bhtk->bhtv', c_state, q))
    den = jnp.sum(s, axis=-1) + w_inter * jnp.einsum('bhk,bhtk->bht', n_state, q)
    h = num / jnp.maximum(jnp.abs(den), jnp.exp(-m_t))[..., None]
    b_last = b[..., -1]
    g = b_last[..., None] - b + li
    m_new = jnp.maximum(b_last + m_state, jnp.max(g, axis=-1))
    decay = jnp.exp(b_last + m_state - m_new)
    w_state = jnp.exp(g - m_new[..., None])
    c_new = decay[..., None, None] * c_state + jnp.einsum('bhs,bhsv,bhsk->bhvk', w_state, v, k)
    n_new = decay[..., None] * n_state + jnp.einsum('bhs,bhsk->bhk', w_state, k)
    return (c_new, n_new, m_new), h


def mlstm_chunkwise(q, k, v, i_pre, f_pre):
    bsz, nh, seq, dh = q.shape
    nc = seq // ML_CHUNK
    li = i_pre
    lf = jax.nn.log_sigmoid(f_pre)

    def to_chunks(t):
        return jnp.moveaxis(t.reshape(bsz, nh, nc, ML_CHUNK, *t.shape[3:]), 2, 0)

    xs = (to_chunks(q), to_chunks(k), to_chunks(v), to_chunks(li), to_chunks(lf))
    init = (jnp.zeros((bsz, nh, dh, dh), jnp.float32),
            jnp.zeros((bsz, nh, dh), jnp.float32),
            jnp.zeros((bsz, nh), jnp.float32))
    _, hs = lax.scan(_mlstm_chunk_step, init, xs)
    return jnp.moveaxis(hs, 0, 2).reshape(bsz, nh, seq, dh)


def _conv_centred(x, w, b):
    y = lax.conv_general_dilated(
        x, w[:, None, :].astype(x.dtype), window_strides=(1,),
        padding=[(CONV_K // 2, CONV_K // 2)],
        dimension_numbers=('NWC', 'WIO', 'NWC'),
        feature_group_count=x.shape[-1])
    return y + b.astype(x.dtype)


def mlstm_group(x_m, o_pre, gate_pre, gate_bias, conv_w, conv_b, wq, wk, wv, head_norm, skip):
    bsz, seq, _ = x_m.shape
    f32 = jnp.float32
    xc = jax.nn.silu(_conv_centred(x_m, conv_w, conv_b))
    xc_h = xc.astype(f32).reshape(bsz, seq, ML_HEADS, ML_DH)
    xm_h = x_m.astype(f32).reshape(bsz, seq, ML_HEADS, ML_DH)
    q = jnp.einsum('bshd,hde->bhse', xc_h, wq.astype(f32))
    k = jnp.einsum('bshd,hde->bhse', xc_h, wk.astype(f32)) * (ML_DH ** -0.5)
    v = jnp.einsum('bshd,hde->bhse', xm_h, wv.astype(f32))
    gp = jnp.transpose(gate_pre.astype(f32) + gate_bias.astype(f32), (2, 0, 3, 1))
    h_fwd = mlstm_chunkwise(q, k, v, gp[0], gp[2])
    flip = lambda t: jnp.flip(t, axis=2)
    h_bwd = flip(mlstm_chunkwise(flip(q), flip(k), flip(v), flip(gp[1]), flip(gp[3])))
    h = h_fwd + h_bwd
    mu = jnp.mean(h, axis=-1, keepdims=True)
    var = jnp.mean(jnp.square(h - mu), axis=-1, keepdims=True)
    hn = (h - mu) * lax.rsqrt(var + EPS)
    hn = jnp.transpose(hn, (0, 2, 1, 3)).reshape(bsz, seq, D_ML) * head_norm.astype(f32)
    out = jax.nn.sigmoid(o_pre.astype(f32)) * (hn + skip.astype(f32) * xc.astype(f32))
    return out.astype(x_m.dtype)


def hybrid_layer(x, norm_mix_pre, norm_mix_post, norm_ffn_pre, norm_ffn_post, w_in, ml_gate_bias,
                 s5_lam_re, s5_lam_im, s5_log_dt, s5_b_re, s5_b_im, s5_c_re, s5_c_im, s5_d,
                 s5_w_glu, s5_b_glu, ml_conv_w, ml_conv_b, ml_wq, ml_wk, ml_wv, ml_head_norm,
                 ml_skip, w_out, w_gate, w_up, w_down):
    bsz, seq, _ = x.shape
    h = rmsnorm(x, norm_mix_pre)
    proj = h @ w_in.astype(h.dtype)
    u_s5 = proj[..., :D_S5]
    x_m = proj[..., D_S5:D_S5 + D_ML]
    o_pre = proj[..., D_S5 + D_ML:D_S5 + 2 * D_ML]
    gate_pre = proj[..., D_S5 + 2 * D_ML:].reshape(bsz, seq, N_GATES, ML_HEADS)
    y_s5 = s5_group(u_s5, s5_lam_re, s5_lam_im, s5_log_dt, s5_b_re, s5_b_im,
                    s5_c_re, s5_c_im, s5_d, s5_w_glu, s5_b_glu)
    y_ml = mlstm_group(x_m, o_pre, gate_pre, ml_gate_bias, ml_conv_w, ml_conv_b,
                       ml_wq, ml_wk, ml_wv, ml_head_norm, ml_skip)
    y = jnp.concatenate([y_s5, y_ml], axis=-1) @ w_out.astype(x.dtype)
    x = x + rmsnorm(y, norm_mix_post)
    h = rmsnorm(x, norm_ffn_pre)
    f = (jax.nn.silu(h @ w_gate.astype(h.dtype)) * (h @ w_up.astype(h.dtype))) @ w_down.astype(h.dtype)
    return x + rmsnorm(f, norm_ffn_post)


def setup_inputs(seed: int = 0) -> dict:
    key = jax.random.key(seed)
    ks = jax.random.split(key, 32)
    f32 = jnp.float32
    nrm = lambda k, shape, scale: jax.random.normal(k, shape, f32) * scale
    gain = lambda k, n: 1.0 + 0.05 * jax.random.normal(k, (DEPTH, n), f32)
    x = jax.random.normal(ks[0], (BATCH, SEQ, D_MODEL), f32)
    gb_noise = 0.1 * jax.random.normal(ks[6], (DEPTH, N_GATES, ML_HEADS), f32)
    f_lin = jnp.linspace(3.0, 6.0, ML_HEADS, dtype=f32)
    gate_offset = jnp.stack([jnp.zeros_like(f_lin), jnp.zeros_like(f_lin), f_lin, f_lin], axis=0)
    ml_gate_bias = gb_noise + gate_offset[None]
    n_idx = jnp.arange(S5_P, dtype=f32)
    s5_lam_re = -0.5 + 0.01 * jax.random.normal(ks[7], (DEPTH, 2, S5_NG, S5_P), f32)
    s5_lam_im = math.pi * n_idx + 0.01 * jax.random.normal(ks[8], (DEPTH, 2, S5_NG, S5_P), f32)
    s5_log_dt = jax.random.uniform(ks[9], (DEPTH, 2, S5_NG), f32,
                                   minval=math.log(1e-3), maxval=math.log(1e-1))
    return {
        'x': x,
        'norm_mix_pre': gain(ks[1], D_MODEL),
        'norm_mix_post': gain(ks[2], D_MODEL),
        'norm_ffn_pre': gain(ks[3], D_MODEL),
        'norm_ffn_post': gain(ks[4], D_MODEL),
        'w_in': nrm(ks[5], (DEPTH, D_MODEL, D_IN), D_MODEL ** -0.5),
        'ml_gate_bias': ml_gate_bias,
        's5_lam_re': s5_lam_re,
        's5_lam_im': s5_lam_im,
        's5_log_dt': s5_log_dt,
        's5_b_re': nrm(ks[10], (DEPTH, S5_NG, S5_P, S5_GROUP), (2 * S5_GROUP) ** -0.5),
        's5_b_im': nrm(ks[11], (DEPTH, S5_NG, S5_P, S5_GROUP), (2 * S5_GROUP) ** -0.5),
        's5_c_re': nrm(ks[12], (DEPTH, S5_NG, S5_GROUP, S5_P), (2 * S5_P) ** -0.5),
        's5_c_im': nrm(ks[13], (DEPTH, S5_NG, S5_GROUP, S5_P), (2 * S5_P) ** -0.5),
        's5_d': nrm(ks[14], (DEPTH, D_S5), 1.0),
        's5_w_glu': nrm(ks[15], (DEPTH, D_S5, D_S5), D_S5 ** -0.5),
        's5_b_glu': nrm(ks[16], (DEPTH, D_S5), 0.02),
        'ml_conv_w': nrm(ks[17], (DEPTH, CONV_K, D_ML), CONV_K ** -0.5),
        'ml_conv_b': nrm(ks[18], (DEPTH, D_ML), 0.02),
        'ml_wq': nrm(ks[19], (DEPTH, ML_HEADS, ML_DH, ML_DH), ML_DH ** -0.5),
        'ml_wk': nrm(ks[20], (DEPTH, ML_HEADS, ML_DH, ML_DH), ML_DH ** -0.5),
        'ml_wv': nrm(ks[21], (DEPTH, ML_HEADS, ML_DH, ML_DH), ML_DH ** -0.5),
        'ml_head_norm': gain(ks[22], D_ML),
        'ml_skip': 1.0 + 0.05 * jax.random.normal(ks[23], (DEPTH, D_ML), f32),
        'w_out': nrm(ks[24], (DEPTH, D_MIX, D_MODEL), D_MIX ** -0.5),
        'w_gate': nrm(ks[25], (DEPTH, D_MODEL, D_FF), D_MODEL ** -0.5),
        'w_up': nrm(ks[26], (DEPTH, D_MODEL, D_FF), D_MODEL ** -0.5),
        'w_down': nrm(ks[27], (DEPTH, D_FF, D_MODEL), D_FF ** -0.5),
    }


def reference(x, norm_mix_pre, norm_mix_post, norm_ffn_pre, norm_ffn_post, w_in, ml_gate_bias,
              s5_lam_re, s5_lam_im, s5_log_dt, s5_b_re, s5_b_im, s5_c_re, s5_c_im, s5_d,
              s5_w_glu, s5_b_glu, ml_conv_w, ml_conv_b, ml_wq, ml_wk, ml_wv, ml_head_norm,
              ml_skip, w_out, w_gate, w_up, w_down):
    for l in range(DEPTH):
        x = hybrid_layer(
            x, norm_mix_pre[l], norm_mix_post[l], norm_ffn_pre[l], norm_ffn_post[l], w_in[l],
            ml_gate_bias[l], s5_lam_re[l], s5_lam_im[l], s5_log_dt[l], s5_b_re[l], s5_b_im[l],
            s5_c_re[l], s5_c_im[l], s5_d[l], s5_w_glu[l], s5_b_glu[l], ml_conv_w[l], ml_conv_b[l],
            ml_wq[l], ml_wk[l], ml_wv[l], ml_head_norm[l], ml_skip[l], w_out[l], w_gate[l],
            w_up[l], w_down[l])
    return x
```

```python
import math
from contextlib import ExitStack

import numpy as np
import concourse.bass as bass
import concourse.mybir as mybir
from concourse.bass_utils import run_bass_kernel_spmd

F32 = mybir.dt.float32
BF = mybir.dt.bfloat16
I32 = mybir.dt.int32
AF = mybir.ActivationFunctionType
ALU = mybir.AluOpType

D = 1024
D_S5 = 512
D_IN = 1552
D_FF = 2816
NFF = D_FF // 128
EPS = 1e-6
TWO_PI = 2.0 * math.pi


class Prog:
    COMPUTE = ("pe", "act", "dve", "pool")

    def __init__(self, nc, same_engine_sync=True):
        self.nc = nc
        self.ops = []
        self.same_engine_sync = same_engine_sync

    def op(self, eng, fn, reads=(), writes=()):
        ps_r = tuple(r for r in reads if isinstance(r, str) and r.startswith("ps") and r[2:].isdigit())
        if ps_r:
            reads = tuple(r for r in reads if r not in ps_r)
            writes = tuple(writes) + tuple(r for r in ps_r if r not in writes)
        self.ops.append(dict(kind="c", eng=eng, fn=fn, reads=tuple(reads), writes=tuple(writes)))

    def dma(self, queue, stream, fn, reads=(), writes=()):
        self.ops.append(dict(kind="d", eng=queue, stream=stream, fn=fn,
                             reads=tuple(reads), writes=tuple(writes)))

    def fence(self, eng, reads):
        self.ops.append(dict(kind="f", eng=eng, fn=None, reads=tuple(reads), writes=()))

    def barrier(self):
        for e in ("pe", "act", "dve", "pool", "sp"):
            self.ops.append(dict(kind="b", eng=e, fn=None, reads=(), writes=()))

    def emit(self, stack):
        nc = self.nc
        ops = self.ops
        last_w = {}
        readers = {}
        deps = []
        needed = set()
        last_of = {}
        for i, o in enumerate(ops):
            d = set()
            if o["kind"] == "b":
                d = set(last_of.values())
            for r in o["reads"]:
                if r in last_w:
                    d.add(last_w[r])
            for w in o["writes"]:
                if w in last_w:
                    d.add(last_w[w])
                for rr in readers.get(w, ()):
                    d.add(rr)
            d.discard(i)
            for r in o["reads"]:
                readers.setdefault(r, []).append(i)
            for w in o["writes"]:
                last_w[w] = i
                readers[w] = []
            if o["kind"] == "c":
                last_of[o["eng"]] = i
            elif o["kind"] == "d":
                last_of[("d", o["stream"])] = i
            dd = set()
            for j in d:
                oj = ops[j]
                if oj["kind"] in ("f", "b"):
                    continue
                if oj["kind"] == "c" and o["kind"] == "c" and oj["eng"] == o["eng"]:
                    if o["eng"] == "pe" or not self.same_engine_sync:
                        continue
                if oj["kind"] == "c" and o["kind"] == "b" and oj["eng"] == o["eng"]:
                    continue
                dd.add(j)
            deps.append(dd)
            needed |= dd
        eng_sem = {}
        for e in self.COMPUTE:
            eng_sem[e] = stack.enter_context(nc.semaphore("sem_" + e))
        stream_sem = {}
        counts = {}
        sig = {}
        for i, o in enumerate(ops):
            if o["kind"] == "c":
                if i in needed:
                    counts[o["eng"]] = counts.get(o["eng"], 0) + 1
                    sig[i] = (o["eng"], counts[o["eng"]])
            elif o["kind"] == "d":
                s = o["stream"]
                if s not in stream_sem:
                    stream_sem[s] = stack.enter_context(nc.semaphore("sd_%d" % len(stream_sem)))
                counts[("d", s)] = counts.get(("d", s), 0) + 16
                sig[i] = (("d", s), counts[("d", s)])
        self.counts = counts

        def semof(k):
            return eng_sem[k] if not isinstance(k, tuple) else stream_sem[k[1]]

        per_eng = {}
        for i, o in enumerate(ops):
            per_eng.setdefault(o["eng"], []).append(i)

        block = stack.enter_context(nc.Block())
        handles = {"pe": block.tensor, "act": block.scalar, "dve": block.vector,
                   "pool": block.gpsimd, "sp": block.sync}

        def make(idxs):
            def body(eng):
                waited = {}
                for i in idxs:
                    o = ops[i]
                    need = {}
                    for j in deps[i]:
                        k, v = sig[j]
                        if need.get(k, 0) < v:
                            need[k] = v
                    for k, v in need.items():
                        if waited.get(k, 0) >= v:
                            continue
                        eng.wait_ge(semof(k), v)
                        waited[k] = v
                    if o["kind"] in ("f", "b"):
                        continue
                    ins = o["fn"](eng)
                    if i in sig:
                        k, v = sig[i]
                        ins.then_inc(semof(k), 16 if isinstance(k, tuple) else 1)
            return body

        for engname, idxs in per_eng.items():
            handles[engname](make(idxs))


class Arena:
    def __init__(self, base_ap, nwords):
        self.base = base_ap
        self.n = nwords
        self.off = 0

    def alloc(self, shape, dtype):
        nel = 1
        for s in shape:
            nel *= s
        nbytes = nel * (4 if dtype in (F32, I32) else 2)
        nw = (nbytes + 3) // 4
        nw = (nw + 7) // 8 * 8
        assert self.off + nw <= self.n, ("arena overflow", self.off, nw, self.n)
        ap = self.base[:, self.off:self.off + nw]
        self.off += nw
        if dtype != F32:
            ap = ap.bitcast(dtype)
        ap = ap[:, 0:nel]
        if len(shape) == 2:
            ap = ap.rearrange("p (a b) -> p a b", b=shape[1])
        elif len(shape) == 3:
            ap = ap.rearrange("p (a b c) -> p a b c", b=shape[1], c=shape[2])
        elif len(shape) == 4:
            ap = ap.rearrange("p (a b c d) -> p a b c d", b=shape[1], c=shape[2], d=shape[3])
        return ap


def build_program(nseq, S, taps=()):
    assert S % 1024 == 0
    NT = S // 128
    NCH = S // 8
    NHB = S // 1024
    NTB = S // 512
    NTOK = nseq * S

    nc = bass.Bass("TRN2", target_bir_lowering=False)
    dt_in = {}

    def din(name, shape):
        dt_in[name] = nc.dram_tensor(name, list(shape), F32, kind="ExternalInput").ap()
        return dt_in[name]

    x = din("x", [NTOK, D])
    g_mix_pre = din("norm_mix_pre", [D])
    g_mix_post = din("norm_mix_post", [D])
    g_ffn_pre = din("norm_ffn_pre", [D])
    g_ffn_post = din("norm_ffn_post", [D])
    w_in = din("w_in", [D, D_IN])
    gate_bias = din("ml_gate_bias", [16])
    lam_re = din("s5_lam_re", [64, 64])
    lam_im = din("s5_lam_im", [64, 64])
    log_dt = din("s5_log_dt", [64])
    b_re = din("s5_b_re", [32, 64, 16])
    b_im = din("s5_b_im", [32, 64, 16])
    c_re = din("s5_c_re", [512, 64])
    c_im = din("s5_c_im", [512, 64])
    s5_d = din("s5_d", [512])
    w_glu = din("s5_w_glu", [512, 512])
    b_glu = din("s5_b_glu", [512])
    conv_w = din("ml_conv_w", [5, 512])
    conv_b = din("ml_conv_b", [512])
    wq = din("ml_wq", [4, 128, 128])
    wk = din("ml_wk", [4, 128, 128])
    wv = din("ml_wv", [4, 128, 128])
    head_norm = din("ml_head_norm", [512])
    skip = din("ml_skip", [512])
    w_out = din("w_out", [D, D])
    w_gate = din("w_gate", [D, D_FF])
    w_up = din("w_up", [D, D_FF])
    w_down = din("w_down", [D_FF, D])
    out = nc.dram_tensor("out", [NTOK, D], F32, kind="ExternalOutput").ap()
    x1_d = nc.dram_tensor("x1_scr", [NTOK, D], F32, kind="Internal").ap()
    s5m_d = nc.dram_tensor("s5m_scr", [32, 128, 640], BF, kind="Internal").ap()
    winb_d = nc.dram_tensor("winb_scr", [8, 128, D_IN], BF, kind="Internal").ap()
    woutb_d = nc.dram_tensor("woutb_scr", [8, 128, D], BF, kind="Internal").ap()
    tap_out = {}
    for tname, tshape in taps:
        tap_out[tname] = nc.dram_tensor("tap_" + tname, list(tshape), F32, kind="ExternalOutput").ap()

    st = ExitStack()
    AW = 53200
    arena_t = st.enter_context(nc.sbuf_tensor("arena", [128, AW], F32))
    AR = Arena(arena_t[:], AW)
    psb = [st.enter_context(nc.psum_tensor("ps%d" % i, [128, 512], F32)) for i in range(8)]
    P = Prog(nc)

    def ps(i):
        return psb[i][:]

    def psbf(i):
        return psb[i][:].bitcast(BF)

    ident_b = AR.alloc([128], BF)
    gpost2 = AR.alloc([D], F32)
    gfpre_c = AR.alloc([8], F32)
    eps_c = AR.alloc([1], F32)
    persist_end = AR.off
    ident_f = AR.alloc([128], F32)
    ones_f = AR.alloc([128], F32)
    tri_f = AR.alloc([128], F32)
    tri_b = AR.alloc([128], F32)
    msk_f = AR.alloc([128], BF)
    msk_b = AR.alloc([128], BF)
    gpost = AR.alloc([D], F32)
    gbias = AR.alloc([16], F32)
    gpre_c = AR.alloc([8], F32)
    bglu_c = AR.alloc([4], F32)
    convw_c = AR.alloc([4, 5], F32)
    convb_c = AR.alloc([4], F32)
    hnorm_c = AR.alloc([4], F32)
    skip_c = AR.alloc([4], F32)
    one_c = AR.alloc([1], F32)
    coefA = AR.alloc([32], F32)
    coefB = AR.alloc([2, 32], F32)
    wglub = AR.alloc([4, 512], BF)
    wqb = AR.alloc([4, 128], BF)
    wkb = AR.alloc([4, 128], BF)
    wvb = AR.alloc([4, 128], BF)
    dgc = AR.alloc([4, 5, 128], BF)

    def V(eng, fn, reads, writes):
        P.op(eng, fn, reads, writes)

    def dma_in(stream, dst, src, writes, reads=(), slow=False):
        P.dma("sp", stream, lambda e: e.dma_start(out=dst, in_=src, allow_slow_non_contiguous=slow),
              reads=reads, writes=writes)

    V("pool", lambda e: e.memset(ones_f, 1.0), [], ["ones_f"])
    V("pool", lambda e: e.memset(eps_c, EPS), [], ["eps_c"])
    V("pool", lambda e: e.memset(one_c, 1.0), [], ["one_c"])
    V("pool", lambda e: e.affine_select(out=ident_f, in_=ones_f, pattern=[[-1, 128]], compare_op=ALU.is_equal,
                                        fill=0.0, base=0, channel_multiplier=1), ["ones_f"], ["ident_f"])
    V("pool", lambda e: e.affine_select(out=tri_f, in_=ones_f, pattern=[[1, 128]], compare_op=ALU.is_ge,
                                        fill=0.0, base=0, channel_multiplier=-1), ["ones_f"], ["tri_f"])
    V("pool", lambda e: e.affine_select(out=tri_b, in_=ones_f, pattern=[[-1, 128]], compare_op=ALU.is_ge,
                                        fill=0.0, base=0, channel_multiplier=1), ["ones_f"], ["tri_b"])
    V("dve", lambda e: e.tensor_copy(out=ident_b, in_=ident_f), ["ident_f"], ["ident_b"])
    V("dve", lambda e: e.tensor_copy(out=msk_f, in_=tri_f), ["tri_f"], ["msk_f"])
    V("dve", lambda e: e.tensor_copy(out=msk_b, in_=tri_b), ["tri_b"], ["msk_b"])
    dma_in("c0", gpost, g_mix_post.partition_broadcast(128), ["gpost"])
    dma_in("c1", gpost2, g_ffn_post.partition_broadcast(128), ["gpost2"])
    dma_in("c2", gbias, gate_bias.partition_broadcast(128), ["gbias"])
    dma_in("c3", gpre_c, g_mix_pre.rearrange("(c p) -> p c", p=128), ["gpre_c"], slow=True)
    dma_in("c4", gfpre_c, g_ffn_pre.rearrange("(c p) -> p c", p=128), ["gfpre_c"], slow=True)
    dma_in("c5", bglu_c, b_glu.rearrange("(c p) -> p c", p=128), ["bglu_c"], slow=True)
    dma_in("c6", convb_c, conv_b.rearrange("(c p) -> p c", p=128), ["convb_c"], slow=True)
    dma_in("c7", hnorm_c, head_norm.rearrange("(c p) -> p c", p=128), ["hnorm_c"], slow=True)
    dma_in("c8", skip_c, skip.rearrange("(c p) -> p c", p=128), ["skip_c"], slow=True)
    for k in range(5):
        dma_in("c9", convw_c[:, :, k], conv_w[k].rearrange("(c p) -> p c", p=128), ["convw_c%d" % k], slow=True)
    CONVW = ["convw_c%d" % k for k in range(5)]

    stg = AR.alloc([2, D_IN], F32)

    def load_convert(src_rows, ncols, dst, scale_col, tag, keyw, eng_cycle=("act", "dve")):
        for i, src in enumerate(src_rows):
            sl = i % 2
            sap = stg[:, sl, 0:ncols]
            dma_in("stg%d" % sl, sap, src, ["stg%d" % sl])
            d_ap = dst(i)
            sc = scale_col(i) if scale_col is not None else None
            en = eng_cycle[i % len(eng_cycle)]
            if en == "act":
                if sc is None:
                    V("act", lambda e, d_ap=d_ap, sap=sap: e.activation(out=d_ap, in_=sap, func=AF.Copy),
                      ["stg%d" % sl], [keyw])
                else:
                    V("act", lambda e, d_ap=d_ap, sap=sap, sc=sc: e.activation(out=d_ap, in_=sap, func=AF.Copy, scale=sc),
                      ["stg%d" % sl, "gpre_c", "gfpre_c"], [keyw])
            else:
                if sc is None:
                    V("dve", lambda e, d_ap=d_ap, sap=sap: e.tensor_copy(out=d_ap, in_=sap), ["stg%d" % sl], [keyw])
                else:
                    V("dve", lambda e, d_ap=d_ap, sap=sap, sc=sc: e.tensor_scalar(out=d_ap, in0=sap, scalar1=sc, scalar2=None, op0=ALU.mult),
                      ["stg%d" % sl, "gpre_c", "gfpre_c"], [keyw])

    load_convert([w_glu[c * 128:(c + 1) * 128, :] for c in range(4)], 512, lambda i: wglub[:, i, :], None, "wglu", "wglub")
    load_convert([wq[h] for h in range(4)], 128, lambda i: wqb[:, i, :], None, "wq", "wqb")
    for h in range(4):
        sl = h % 2
        sap = stg[:, sl, 0:128]
        dma_in("stg%d" % sl, sap, wk[h], ["stg%d" % sl])
        V("act", lambda e, h=h, sap=sap: e.activation(out=wkb[:, h, :], in_=sap, func=AF.Copy, scale=float(128 ** -0.5)),
          ["stg%d" % sl], ["wkb"])
    load_convert([wv[h] for h in range(4)], 128, lambda i: wvb[:, i, :], None, "wv", "wvb")

    mark = AR.off
    cvb = AR.alloc([2, D_IN], BF)
    Lin = AR.alloc([2, 128], F32)
    LRt = AR.alloc([64], F32)
    LIt = AR.alloc([64], F32)
    DTt = AR.alloc([64], F32)
    lrd = AR.alloc([64], F32)
    ang = AR.alloc([64], F32)
    jv = AR.alloc([16], F32)
    Fm = AR.alloc([64, 16], F32)
    Fs = AR.alloc([64, 16], F32)
    Fc = AR.alloc([64, 16], F32)
    Fi = AR.alloc([64, 16], I32)
    Ff = AR.alloc([64, 16], F32)
    Fg = AR.alloc([64, 16], F32)
    Wre = AR.alloc([64, 16], F32)
    Wim = AR.alloc([64, 16], F32)
    t64a = AR.alloc([64], F32)
    t64b = AR.alloc([64], F32)
    t64c = AR.alloc([64], F32)
    fr = AR.alloc([64], F32)
    fi = AR.alloc([64], F32)
    Gre = AR.alloc([64, 8], F32)
    Gim = AR.alloc([64, 8], F32)
    Gt = AR.alloc([64, 8], F32)
    m0 = AR.alloc([1], F32)
    m1 = AR.alloc([1], F32)
    nm0 = AR.alloc([1], F32)
    nm1 = AR.alloc([1], F32)
    XA = AR.alloc([64, 8], F32)
    XB = AR.alloc([64, 8], F32)
    Bre_t = AR.alloc([32, 16], F32)
    Bim_t = AR.alloc([32, 16], F32)
    Cin = AR.alloc([4, 2, 128], F32)
    Cre_t = AR.alloc([32, 16], F32)
    Cim_t = AR.alloc([32, 16], F32)
    Dcol = AR.alloc([32], F32)
    bmf = AR.alloc([8, 16], F32)
    bmb = AR.alloc([8, 16], F32)
    Pst = AR.alloc([2, 16, 8, 16], F32)
    Hst = AR.alloc([2, 16, 8, 16], F32)
    Qre_s = AR.alloc([16, 8, 16], F32)
    Qim_s = AR.alloc([16, 8, 16], F32)
    Pre_s = AR.alloc([16, 8, 16], F32)
    Pim_s = AR.alloc([16, 8, 16], F32)
    tmpA = AR.alloc([16, 8, 16], F32)
    tmpB = AR.alloc([16, 8, 16], F32)
    s5o = AR.alloc([2, 5, 128], BF)
    RW = AR.alloc([4, 16, 8], F32)
    tT1 = AR.alloc([128], F32)
    tT2 = AR.alloc([128], F32)

    def tt(eng, o, a, b, op, reads, writes):
        V(eng, lambda e: e.tensor_tensor(out=o, in0=a, in1=b, op=op), reads, writes)

    def ts(eng, o, a, s1, s2, op0, op1, reads, writes):
        if op1 is None:
            V(eng, lambda e: e.tensor_scalar(out=o, in0=a, scalar1=s1, scalar2=None, op0=op0), reads, writes)
        else:
            V(eng, lambda e: e.tensor_scalar(out=o, in0=a, scalar1=s1, scalar2=s2, op0=op0, op1=op1), reads, writes)

    def act(o, a, func, reads, writes, scale=None, bias=None, accum=None, eng="act"):
        kw = {}
        if scale is not None:
            kw["scale"] = scale
        if bias is not None:
            kw["bias"] = bias
        if accum is not None:
            kw["accum_out"] = accum
        V(eng, lambda e: e.activation(out=o, in_=a, func=func, **kw), reads, writes)

    def stt(eng, o, a, s, b, op0, op1, reads, writes):
        V(eng, lambda e: e.scalar_tensor_tensor(out=o, in0=a, scalar=s, in1=b, op0=op0, op1=op1), reads, writes)

    def cp(eng, o, a, reads, writes):
        if eng == "act":
            V(eng, lambda e: e.activation(out=o, in_=a, func=AF.Copy), reads, writes)
        else:
            V(eng, lambda e: e.tensor_copy(out=o, in_=a), reads, writes)

    def mm(o, lhsT, rhs, start, stop, reads, writes):
        V("pe", lambda e: e.matmul(o, lhsT, rhs, start=start, stop=stop), reads, writes)

    def tp(o, in_, ident, reads, writes):
        V("pe", lambda e: e.transpose(o, in_, ident), reads, writes)

    for h in range(4):
        for k in range(5):
            ts("dve", dgc[:, h, k, :], ident_f, convw_c[:, h, k:k + 1], None, ALU.mult, None, ["ident_f"] + CONVW, ["dgc"])

    for ri, src in enumerate((lam_re, lam_im)):
        for dup in range(2):
            dma_in("pl%d%d" % (ri, dup), Lin[0:64, ri, dup * 64:(dup + 1) * 64], src, ["Lin"])
    dma_in("pl_dt", DTt, log_dt.partition_broadcast(128), ["DTt"])
    tp(ps(0)[:, 0:64], Lin[0:64, 0, :], ident_f[0:64, 0:64], ["Lin", "ident_f"], ["ps0"])
    tp(ps(0)[:, 64:128], Lin[0:64, 1, :], ident_f[0:64, 0:64], ["Lin", "ident_f"], ["ps0"])
    cp("dve", LRt, ps(0)[:, 0:64], ["ps0"], ["LRt"])
    cp("dve", LIt, ps(0)[:, 64:128], ["ps0"], ["LIt"])
    act(DTt, DTt, AF.Exp, ["DTt"], ["DTt"])
    tt("dve", lrd, LRt, DTt, ALU.mult, ["LRt", "DTt"], ["lrd"])
    tt("dve", ang, LIt, DTt, ALU.mult, ["LIt", "DTt"], ["ang"])
    ts("dve", ang, ang, 1.0 / TWO_PI, None, ALU.mult, None, ["ang"], ["ang"])
    for jj in range(16):
        V("pool", lambda e, jj=jj: e.memset(jv[:, jj:jj + 1], float(jj - 7)), [], ["jv"])
    V("pool", lambda e: e.memset(m0, 0.0), [], ["m0"])
    V("pool", lambda e: e.memset(m0[0:64, :], 1.0), ["m0"], ["m0"])
    V("pool", lambda e: e.memset(m1, 1.0), [], ["m1"])
    V("pool", lambda e: e.memset(m1[0:64, :], 0.0), ["m1"], ["m1"])
    ts("dve", nm0, m0, -1.0, None, ALU.mult, None, ["m0"], ["nm0"])
    ts("dve", nm1, m1, -1.0, None, ALU.mult, None, ["m1"], ["nm1"])
    jvb = jv[:, None, :].broadcast_to([128, 64, 16])
    tt("dve", Fm, lrd[:, :, None].broadcast_to([128, 64, 16]), jvb, ALU.mult, ["lrd", "jv"], ["Fm"])
    tt("dve", Fs, ang[:, :, None].broadcast_to([128, 64, 16]), jvb, ALU.mult, ["ang", "jv"], ["Fs"])
    ts("dve", Fc, Fs, 0.25, None, ALU.add, None, ["Fs"], ["Fc"])
    act(Fm, Fm, AF.Exp, ["Fm"], ["Fm"])

    def range_reduce(Fx, key):
        cp("dve", Fi, Fx, [key], ["Fi"])
        cp("dve", Ff, Fi, ["Fi"], ["Ff"])
        tt("dve", Fx, Fx, Ff, ALU.subtract, [key, "Ff"], [key])
        ts("dve", Fg, Fx, 0.5, None, ALU.is_gt, None, [key], ["Fg"])
        tt("dve", Fx, Fx, Fg, ALU.subtract, [key, "Fg"], [key])
        ts("dve", Fg, Fx, -0.5, None, ALU.is_lt, None, [key], ["Fg"])
        tt("dve", Fx, Fx, Fg, ALU.add, [key, "Fg"], [key])

    range_reduce(Fs, "Fs")
    range_reduce(Fc, "Fc")
    SIN_SCALE = TWO_PI * (1.0 - 2e-6)
    act(Fs, Fs, AF.Sin, ["Fs"], ["Fs"], scale=SIN_SCALE)
    act(Fc, Fc, AF.Sin, ["Fc"], ["Fc"], scale=SIN_SCALE)
    tt("dve", Wre, Fm, Fc, ALU.mult, ["Fm", "Fc"], ["Wre"])
    tt("dve", Wim, Fm, Fs, ALU.mult, ["Fm", "Fs"], ["Wim"])
    ts("dve", t64a, Wre[:, :, 8], -1.0, None, ALU.add, None, ["Wre"], ["t64a"])
    tt("dve", t64b, LRt, LRt, ALU.mult, ["LRt"], ["t64b"])
    tt("dve", t64c, LIt, LIt, ALU.mult, ["LIt"], ["t64c"])
    tt("dve", t64b, t64b, t64c, ALU.add, ["t64b", "t64c"], ["t64b"])
    V("dve", lambda e: e.reciprocal(out=t64b, in_=t64b), ["t64b"], ["t64b"])
    tt("dve", fr, t64a, LRt, ALU.mult, ["t64a", "LRt"], ["fr"])
    tt("dve", t64c, Wim[:, :, 8], LIt, ALU.mult, ["Wim", "LIt"], ["t64c"])
    tt("dve", fr, fr, t64c, ALU.add, ["fr", "t64c"], ["fr"])
    tt("dve", fr, fr, t64b, ALU.mult, ["fr", "t64b"], ["fr"])
    tt("dve", fi, Wim[:, :, 8], LRt, ALU.mult, ["Wim", "LRt"], ["fi"])
    tt("dve", t64c, t64a, LIt, ALU.mult, ["t64a", "LIt"], ["t64c"])
    tt("dve", fi, fi, t64c, ALU.subtract, ["fi", "t64c"], ["fi"])
    tt("dve", fi, fi, t64b, ALU.mult, ["fi", "t64b"], ["fi"])
    frb = fr[:, :, None].broadcast_to([128, 64, 8])
    fib = fi[:, :, None].broadcast_to([128, 64, 8])
    tt("dve", Gre, Wre[:, :, 7:15], frb, ALU.mult, ["Wre", "fr"], ["Gre"])
    tt("dve", Gt, Wim[:, :, 7:15], fib, ALU.mult, ["Wim", "fi"], ["Gt"])
    tt("dve", Gre, Gre, Gt, ALU.subtract, ["Gre", "Gt"], ["Gre"])
    tt("dve", Gim, Wre[:, :, 7:15], fib, ALU.mult, ["Wre", "fi"], ["Gim"])
    tt("dve", Gt, Wim[:, :, 7:15], frb, ALU.mult, ["Wim", "fr"], ["Gt"])
    tt("dve", Gim, Gim, Gt, ALU.add, ["Gim", "Gt"], ["Gim"])
    for r in range(2):
        hp = slice(r * 64, (r + 1) * 64)
        cp("dve", coefA[hp, :], Wre[hp, r * 32:(r + 1) * 32, 15], ["Wre"], ["coefA"])
        cp("dve", coefB[hp, 1, :], Wim[hp, r * 32:(r + 1) * 32, 15], ["Wim"], ["coefB"])
        ts("dve", coefB[hp, 0, :], Wim[hp, r * 32:(r + 1) * 32, 15], -1.0, None, ALU.mult, None, ["Wim"], ["coefB"])
    for dup in range(2):
        dma_in("pb%d" % dup, Bre_t[dup * 64:(dup + 1) * 64], b_re.rearrange("g p c -> p g c"), ["Bre_t"])
        dma_in("pb%d" % (2 + dup), Bim_t[dup * 64:(dup + 1) * 64], b_im.rearrange("g p c -> p g c"), ["Bim_t"])
    for ti in range(4):
        for ri, src in enumerate((c_re, c_im)):
            for dup in range(2):
                dma_in("pc%d%d" % (ri, dup), Cin[:, ti, ri, dup * 64:(dup + 1) * 64], src[ti * 128:(ti + 1) * 128, :], ["Cin"])
    for ti in range(4):
        tp(ps(1)[:, ti * 128:(ti + 1) * 128], Cin[:, ti, 0, :], ident_f, ["Cin", "ident_f"], ["ps1"])
        tp(ps(2)[:, ti * 128:(ti + 1) * 128], Cin[:, ti, 1, :], ident_f, ["Cin", "ident_f"], ["ps2"])
    cp("dve", Cre_t.rearrange("p g c -> p (g c)"), ps(1), ["ps1"], ["Cre_t"])
    cp("dve", Cim_t.rearrange("p g c -> p (g c)"), ps(2), ["ps2"], ["Cim_t"])
    for tau in range(8):
        dma_in("pd", Dcol[tau * 16:(tau + 1) * 16, :], s5_d.rearrange("(g c) -> c g", c=16), ["Dcol"], slow=True)
    V("pool", lambda e: e.memset(tmpA[:, 0, :, :], 1.0), [], ["tmpA"])
    V("pool", lambda e: e.affine_select(out=bmf, in_=tmpA[:, 0, :, :], pattern=[[16, 8], [0, 16]], compare_op=ALU.is_ge,
                                        fill=0.0, base=15, channel_multiplier=-1), ["tmpA"], ["bmf"])
    V("pool", lambda e: e.affine_select(out=bmb, in_=tmpA[:, 0, :, :], pattern=[[-16, 8], [0, 16]], compare_op=ALU.is_ge,
                                        fill=0.0, base=0, channel_multiplier=1), ["tmpA"], ["bmb"])

    def cplx_stack(dst, wre_v, wim_v, ca, cb, m_a, m_b, key, rows=slice(0, 128)):
        xa = XA[rows, 0:16, :]
        xb = XB[rows, 0:16, :]
        ta = tmpA[rows]
        tb = tmpB[rows]
        npart = rows.stop - rows.start

        def lin(o, m, okey):
            s0, s1 = m
            s0 = s0[rows] if not isinstance(s0, float) else s0
            s1 = s1[rows] if not isinstance(s1, float) else s1
            ts("dve", o, wre_v, s0, None, ALU.mult, None, ["Wre", "Wim", "Gre", "Gim", "RW", "m0", "m1", "nm0", "nm1"], [okey])
            stt("dve", o, wim_v, s1, o, ALU.mult, ALU.add, ["Wre", "Wim", "Gre", "Gim", "RW", "m0", "m1", "nm0", "nm1", okey], [okey])

        lin(xa, m_a, "XA")
        lin(xb, m_b, "XB")
        d = dst[rows]
        tt("dve", ta, xa[:, :, :, None].broadcast_to([npart, 16, 8, 16]),
           ca[:, :, None, :].broadcast_to([npart, 16, 8, 16]), ALU.mult,
           ["XA", "Bre_t", "Bim_t", "Cre_t", "Cim_t"], ["tmpA"])
        tt("pool", tb, xb[:, :, :, None].broadcast_to([npart, 16, 8, 16]),
           cb[:, :, None, :].broadcast_to([npart, 16, 8, 16]), ALU.mult,
           ["XB", "Bre_t", "Bim_t", "Cre_t", "Cim_t"], ["tmpB"])
        tt("dve", d, ta, tb, ALU.add, ["tmpA", "tmpB"], [key])

    for which, (src_w, dst_d, ncols) in enumerate(((w_in, winb_d, D_IN), (w_out, woutb_d, D))):
        for c in range(8):
            sl = c % 2
            sap = stg[:, sl, 0:ncols]
            dma_in("stg%d" % sl, sap, src_w[c * 128:(c + 1) * 128, :], ["stg%d" % sl])
            cb_ = cvb[:, sl, 0:ncols]
            if which == 0:
                act(cb_, sap, AF.Copy, ["stg%d" % sl, "gpre_c"], ["cvb%d" % sl], scale=gpre_c[:, c:c + 1])
            else:
                cp("act", cb_, sap, ["stg%d" % sl], ["cvb%d" % sl])
            P.dma("sp", "cvw%d" % sl, lambda e, dst_d=dst_d, c=c, cb_=cb_: e.dma_start(out=dst_d[c], in_=cb_),
                  reads=["cvb%d" % sl], writes=["wscr%d" % which])

    bmf2 = bmf.rearrange("p a b -> p (a b)")
    bmb2 = bmb.rearrange("p a b -> p (a b)")
    A_ = slice(0, 128)
    for gh in range(2):
        g0 = gh * 16
        gg_ = slice(g0, g0 + 16)
        for r in range(2):
            gs = slice(r * 32 + g0, r * 32 + g0 + 16)
            if r == 0:
                pw_re, pw_im = Gre[:, gs, ::-1], Gim[:, gs, ::-1]
                hw_re, hw_im = Wre[:, gs, 0:8], Wim[:, gs, 0:8]
                qw_re, qw_im = Wre[:, gs, 8:16], Wim[:, gs, 8:16]
            else:
                pw_re, pw_im = Gre[:, gs, :], Gim[:, gs, :]
                hw_re, hw_im = Wre[:, gs, 7::-1], Wim[:, gs, 7::-1]
                qw_re, qw_im = Wre[:, gs, 15:7:-1], Wim[:, gs, 15:7:-1]
            cplx_stack(Pst[:, r], pw_re, pw_im, Bre_t[:, gg_], Bim_t[:, gg_], (m0, m1), (m1, nm0), "Pst")
            cplx_stack(Hst[:, r], hw_re, hw_im, Cre_t[:, gg_], Cim_t[:, gg_], (m0, nm1), (nm1, nm0), "Hst")
            hp = slice(r * 64, (r + 1) * 64)
            for ti, tv in enumerate((qw_re, qw_im, pw_re, pw_im)):
                cp("dve", RW[hp, ti], tv[hp], ["Wre", "Wim", "Gre", "Gim"], ["RW"])
        cplx_stack(Qre_s, RW[:, 0], RW[:, 1], Cre_t[:, gg_], Cim_t[:, gg_], (1.0, 0.0), (0.0, -1.0), "Qre_s")
        cplx_stack(Qim_s, RW[:, 0], RW[:, 1], Cre_t[:, gg_], Cim_t[:, gg_], (0.0, -1.0), (-1.0, 0.0), "Qim_s")
        cplx_stack(Pre_s, RW[:, 2], RW[:, 3], Bre_t[:, gg_], Bim_t[:, gg_], (1.0, 0.0), (0.0, -1.0), "Pre_s")
        cplx_stack(Pim_s, RW[:, 2], RW[:, 3], Bre_t[:, gg_], Bim_t[:, gg_], (0.0, 1.0), (1.0, 0.0), "Pim_s")

        for gl in range(16):
            g = g0 + gl
            sl = g % 2
            o5 = s5o[:, sl]
            okey = "s5o%d" % sl
            pf = Pst[:, 0, gl].rearrange("p a b -> p (a b)")
            hf = Hst[:, 0, gl].rearrange("p a b -> p (a b)")
            pb_ = Pst[:, 1, gl].rearrange("p a b -> p (a b)")
            hb_ = Hst[:, 1, gl].rearrange("p a b -> p (a b)")
            mm(ps(3)[:, 0:128], pf, hf, True, True, ["Pst", "Hst"], ["ps3"])
            mm(ps(3)[:, 128:256], pb_, hb_, True, True, ["Pst", "Hst"], ["ps3"])
            tt("dve", tT1, ps(3)[:, 0:128], bmf2, ALU.mult, ["ps3", "bmf"], ["tT1"])
            tt("dve", tT2, ps(3)[:, 128:256], bmb2, ALU.mult, ["ps3", "bmb"], ["tT2"])
            tt("dve", tT1, tT1, tT2, ALU.add, ["tT1", "tT2"], ["tT1"])
            stt("dve", o5[:, 0, :], ident_f, Dcol[:, g:g + 1], tT1, ALU.mult, ALU.add, ["ident_f", "Dcol", "tT1"], [okey])
            srcs = (Pre_s, Pim_s)
            for k in range(2):
                tp(ps(4)[:, k * 128:(k + 1) * 128], srcs[k][:, gl].rearrange("p a b -> p (a b)"), ident_f,
                   ["Pre_s", "Pim_s", "ident_f"], ["ps4"])
            cp("act", o5[:, 1:3, :], ps(4)[:, 0:256].rearrange("p (a b) -> p a b", b=128), ["ps4"], [okey])
            cp("dve", o5[:, 3, :], Qre_s[:, gl].rearrange("p a b -> p (a b)"), ["Qre_s"], [okey])
            cp("dve", o5[:, 4, :], Qim_s[:, gl].rearrange("p a b -> p (a b)"), ["Qim_s"], [okey])
            P.dma("sp", "s5w%d" % sl, lambda e, g=g, o5=o5: e.dma_start(out=s5m_d[g], in_=o5.rearrange("p a b -> p (a b)")),
                  reads=[okey], writes=["s5m_d"])

    P.barrier()
    AR.off = mark

    wslot_0 = AR.off
    wslot = AR.alloc([8, D_IN], BF)
    wslot_1 = AR.off
    stgA = stg
    xt = AR.alloc([2, D], F32)
    xnb2_ = AR.alloc([2, D], BF)
    ssq = AR.alloc([4], F32)
    ssqI = AR.alloc([2], F32)
    regII_0 = AR.off
    big32 = AR.alloc([16384], BF)
    hT = big32[:, 0:8 * S].rearrange("p (c t) -> p c t", t=S)
    VS = big32[:, 0:64 * NCH].rearrange("p (r g k) -> p r g k", g=32, k=NCH)
    XX = AR.alloc([NHB, 8, 512], BF)
    XXg = XX.rearrange("p h t f -> p h (t f)").rearrange("p h (g t c) -> p h g t c", g=32, t=8)
    Ucol = AR.alloc([32, NCH], BF)
    regII_1 = AR.off
    xmT = AR.alloc([4, S + 4], BF)
    sigoT = AR.alloc([4, S], BF)
    yT = AR.alloc([8, S], BF)
    gates = AR.alloc([NT, 16], F32)
    lfn = AR.alloc([NT, 8], F32)
    e1 = AR.alloc([NT, 8], F32)
    e2 = AR.alloc([NT, 8], F32)
    e3 = AR.alloc([NT, 8], F32)
    eBL = AR.alloc([NT, 8], F32)
    _save = AR.off
    AR.off = wslot_0
    s5m = AR.alloc([2, 5, 128], BF)
    Z = AR.alloc([2, 3, 32], F32)
    zt1 = AR.alloc([2, 32], F32)
    zt2 = AR.alloc([2, 32], F32)
    gl_sq2 = AR.alloc([2, NCH], F32)
    gl_in2 = AR.alloc([2, NCH], F32)
    ygc2 = AR.alloc([2, NCH], BF)
    sgt = AR.alloc([4, 512], BF)
    VSn = AR.alloc([2, 2, NCH], BF)
    assert AR.off <= wslot_1
    AR.off = wslot_0
    Sm = AR.alloc([2, 128], BF)
    Cf = AR.alloc([2, 129], F32)
    Cb = AR.alloc([2, 2, 129], BF)
    dnv = AR.alloc([2, 3, NT], F32)
    lnv2 = AR.alloc([2, NT], F32)
    lnv3 = AR.alloc([2, NT], F32)
    hnb_all = AR.alloc([NT, 128], BF)
    otmp = AR.alloc([1024], F32)
    assert AR.off <= wslot_1
    AR.off = regII_0
    cacc = AR.alloc([S], F32)
    xc = AR.alloc([S], BF)
    xcs = AR.alloc([S], BF)
    qT = AR.alloc([S], BF)
    kT = AR.alloc([S], BF)
    ktok = AR.alloc([NT, 128], BF)
    vtall = AR.alloc([NT, 4, 129], BF)
    esc = AR.alloc([NT, 4], F32)
    nd = AR.alloc([2, NT, 129], F32)
    hacc = cacc.rearrange("p (i d) -> p i d", d=128)
    assert AR.off <= regII_1, (AR.off, regII_1)
    AR.off = regII_0
    x1t2 = AR.alloc([2, D], F32)
    ytmp2 = AR.alloc([2, D], F32)
    ssqO = AR.alloc([2, 4], F32)
    AR.off = _save
    phaseA_end = AR.off

    V("pool", lambda e: e.memset(xmT, 0.0), [], ["xmT"])
    VSK = [("VS", k) for k in range(NCH)]

    def rstd_from_ss(ss_ap, n, key):
        act(ss_ap, ss_ap, AF.Sqrt, [key, "eps_c"], [key], scale=1.0 / n, bias=eps_c)
        V("dve", lambda e: e.reciprocal(out=ss_ap, in_=ss_ap), [key], [key])

    def norm_transpose_tile(src_ap, src_key, dstT, dst_key, col0, bank):
        par = bank % 2
        xnb = xnb2_[:, par, :]
        kx, ks = "xnb%d" % par, "ssqI%d" % par
        sq = ssqI[:, par:par + 1]
        act(xnb, src_ap, AF.Square, [src_key], [kx, ks], accum=sq)
        rstd_from_ss(sq, D, ks)
        act(xnb, src_ap, AF.Copy, [src_key, ks], [kx], scale=sq)
        pb = psbf(bank)
        for c in range(8):
            tp(pb[:, c * 128:(c + 1) * 128], xnb[:, c * 128:(c + 1) * 128], ident_b, [kx, "ident_b"], ["ps%d" % bank])
        cp("dve", dstT[:, :, col0:col0 + 128], pb.rearrange("p (c t) -> p c t", t=128), ["ps%d" % bank], [dst_key])

    for b in range(nseq):
        t0 = b * S
        dma_in("winl", wslot, winb_d.rearrange("c p n -> p c n"), ["wslot"], reads=["wscr0"])
        cnt = 0
        for tb in range(NTB):
            hk = ("hT", tb)
            for i in range(tb * 4, tb * 4 + 4):
                sl = i % 2
                dma_in("x%d" % sl, xt[:, sl, :], x[t0 + i * 128:t0 + (i + 1) * 128, :], ["xt%d" % sl])
                norm_transpose_tile(xt[:, sl, :], "xt%d" % sl, hT, hk, i * 128, 2 + (i % 2))
            for h in range(4):
                for which in range(2):
                    bank = cnt % 2
                    cnt += 1
                    col0 = 512 + which * 512 + h * 128
                    for c in range(8):
                        mm(ps(bank), wslot[:, c, col0:col0 + 128], hT[:, c, tb * 512:(tb + 1) * 512], c == 0, c == 7,
                           [hk, "wslot"], ["ps%d" % bank])
                    if which == 0:
                        cp("dve", xmT[:, h, 2 + tb * 512:2 + (tb + 1) * 512], ps(bank), ["ps%d" % bank], ["xmT"])
                    else:
                        act(sigoT[:, h, tb * 512:(tb + 1) * 512], ps(bank), AF.Sigmoid, ["ps%d" % bank], ["sigoT"])
            for i in range(tb * 4, tb * 4 + 4):
                for c in range(8):
                    mm(ps(4)[:, i * 16:(i + 1) * 16], hT[:, c, i * 128:(i + 1) * 128], wslot[:, c, 1536:1552], c == 0, c == 7,
                       [hk, "wslot"], ["ps4"])
            if tb % 2 == 1:
                hb = tb // 2
                hks = [("hT", tb - 1), ("hT", tb)]
                for tau in range(8):
                    bank = tau % 2
                    for c in range(8):
                        mm(ps(bank), hT[:, c, hb * 1024 + tau:(hb + 1) * 1024:8], wslot[:, c, 0:512], c == 0, c == 7,
                           hks + ["wslot"], ["ps%d" % bank])
                    xdst = XXg[:, hb, :, tau, :]
                    psrc = ps(bank).rearrange("p (g c) -> p g c", c=16)
                    if tau % 2 == 0:
                        cp("dve", xdst, psrc, ["ps%d" % bank], ["XX%d" % hb])
                    else:
                        cp("act", xdst, psrc, ["ps%d" % bank], ["XX%d" % hb])
                for gq in range(4):
                    bank = 2 + gq % 2
                    pb = psbf(bank)
                    for gg in range(8):
                        g = gq * 8 + gg
                        tp(pb[:, gg * 128:(gg + 1) * 128], XXg[:, hb, g].rearrange("p a b -> p (a b)"), ident_b,
                           ["XX%d" % hb, "ident_b"], ["ps%d" % bank])
                    cp("dve", Ucol[:, gq * 8:(gq + 1) * 8, hb * 128:(hb + 1) * 128],
                       pb.rearrange("p (g k) -> p g k", k=128), ["ps%d" % bank], ["Ucol"])
        tt("dve", gates, ps(4)[:, 0:NT * 16].rearrange("p (i c) -> p i c", c=16),
           gbias[:, None, :].broadcast_to([128, NT, 16]), ALU.add, ["ps4", "gbias"], ["gates"])
        act(lfn, gates[:, :, 8:16], AF.Exp, ["gates"], ["lfn"], scale=-1.0)
        act(lfn, lfn, AF.Ln, ["lfn", "one_c"], ["lfn"], bias=one_c)
        ts("dve", lfn, lfn, -1.0, None, ALU.mult, None, ["lfn"], ["lfn"])
        for i in range(NT):
            mm(ps(5)[:, i * 16:i * 16 + 4], tri_f, lfn[:, i, 0:4], True, True, ["tri_f", "lfn"], ["ps5"])
            mm(ps(5)[:, i * 16 + 4:i * 16 + 8], tri_b, lfn[:, i, 4:8], True, True, ["tri_b", "lfn"], ["ps5"])
            mm(ps(5)[:, i * 16 + 8:i * 16 + 16], ones_f, lfn[:, i, 0:8], True, True, ["ones_f", "lfn"], ["ps5"])
        p5 = ps(5)[:, 0:NT * 16].rearrange("p (i c) -> p i c", c=16)
        act(e1, p5[:, :, 0:8], AF.Exp, ["ps5"], ["e1"])
        tt("dve", e2, gates[:, :, 0:8], p5[:, :, 0:8], ALU.subtract, ["gates", "ps5"], ["e2"])
        act(e2, e2, AF.Exp, ["e2"], ["e2"])
        act(eBL, p5[:, :, 8:16], AF.Exp, ["ps5"], ["eBL"])
        tt("dve", e3, e2, eBL, ALU.mult, ["e2", "eBL"], ["e3"])

        P.barrier()
        for g in range(32):
            sl = g % 2
            dma_in("s5l%d" % sl, s5m[:, sl].rearrange("p a b -> p (a b)"), s5m_d[g], ["s5m%d" % sl], reads=["s5m_d"])
            bank = g % 2
            mm(ps(bank)[:, 0:NCH], s5m[:, sl, 1, :], Ucol[:, g, :], True, True, ["s5m%d" % sl, "Ucol"], ["ps%d" % bank])
            mm(ps(bank)[:, 256:256 + NCH], s5m[:, sl, 2, :], Ucol[:, g, :], True, True, ["s5m%d" % sl, "Ucol"], ["ps%d" % bank])
            src = ps(bank).rearrange("p (r k) -> p r k", k=256)[:, :, 0:NCH]
            cp("act", VS[0:64, :, g, :], src[0:64], ["ps%d" % bank], ["big32"] + VSK)
            cp("dve", VS[64:128, :, g, :], src[64:128, :, ::-1], ["ps%d" % bank], ["big32"] + VSK)
        Zb = Z.rearrange("p a b c -> p (a b c)").rearrange("p (n s g) -> p n s g", n=3, s=2)
        V("dve", lambda e: e.memset(Zb[:, 0], 0.0), [], ["Z0"])
        Ab = coefA[:, None, :].broadcast_to([128, 2, 32])
        for step in range(NCH):
            zi, zo = step % 3, (step + 1) % 3
            zin, zout = Zb[:, zi], Zb[:, zo]
            tt("dve", zt1, Ab, zin, ALU.mult, ["coefA", "Z%d" % zi], ["zt1"])
            tt("dve", zt2, coefB, zin[:, 1::-1, :], ALU.mult, ["coefB", "Z%d" % zi], ["zt2"])
            tt("dve", zt1, zt1, zt2, ALU.add, ["zt1", "zt2"], ["zt1"])
            tt("dve", zout, zt1, VS[:, :, :, step], ALU.add, ["zt1", ("VS", step)], ["Z%d" % zo])
            cp("act", VS[:, :, :, step], zin, ["Z%d" % zi], [("VS", step)])
        def y_matmuls(g):
            sl = g % 2
            dma_in("s5l%d" % sl, s5m[:, sl].rearrange("p a b -> p (a b)"), s5m_d[g], ["s5m%d" % sl], reads=["s5m_d"])
            bank = g % 2
            py = ps(bank)[:, 0:NCH]
            mm(py, s5m[:, sl, 0, :], Ucol[:, g, :], True, False, ["s5m%d" % sl, "Ucol"], ["ps%d" % bank])
            cp("act", VSn[0:64, sl], VS[0:64, :, g, :], ["big32"] + VSK, ["VSn%d" % sl])
            cp("dve", VSn[64:128, sl], VS[64:128, :, g, ::-1], ["big32"] + VSK, ["VSn%d" % sl])
            mm(py, s5m[:, sl, 3, :], VSn[:, sl, 0, :], False, False, ["s5m%d" % sl, "VSn%d" % sl], ["ps%d" % bank])
            mm(py, s5m[:, sl, 4, :], VSn[:, sl, 1, :], False, True, ["s5m%d" % sl, "VSn%d" % sl], ["ps%d" % bank])

        y_matmuls(0)
        for g in range(32):
            sl = g % 2
            bank = g % 2
            py = ps(bank)[:, 0:NCH]
            if g + 1 < 32:
                y_matmuls(g + 1)
            ygc = ygc2[:, sl, :]
            kyg = "ygc%d" % sl
            act(ygc, py, AF.Gelu_apprx_tanh, ["ps%d" % bank], [kyg])
            pb = psbf(2 + g % 2)
            for hb in range(NHB):
                tp(pb[:, hb * 128:(hb + 1) * 128], ygc[:, hb * 128:(hb + 1) * 128], ident_b, [kyg, "ident_b"], ["ps%d" % (2 + g % 2)])
            for hb in range(NHB):
                cp("dve", XX[:, hb, :, g * 16:(g + 1) * 16],
                   pb[:, hb * 128:(hb + 1) * 128].rearrange("p (a b) -> p a b", b=16), ["ps%d" % (2 + g % 2)], ["XX%d" % hb])
        cnt = 0
        for hb in range(NHB):
            for ft in range(4):
                bank = 2 + cnt % 2
                cnt += 1
                pb = psbf(bank)
                for tau in range(8):
                    tp(pb[:, tau * 128:(tau + 1) * 128], XX[:, hb, tau, ft * 128:(ft + 1) * 128], ident_b,
                       ["XX%d" % hb, "ident_b"], ["ps%d" % bank])
                cp("dve" if cnt % 2 else "act", yT[:, ft, hb * 1024:(hb + 1) * 1024].rearrange("p (k t) -> p t k", t=8),
                   pb.rearrange("p (t k) -> p t k", k=128), ["ps%d" % bank], ["yT"])
        for tb in range(NTB):
            for fo in range(4):
                bank = fo % 2
                for ft in range(4):
                    mm(ps(bank), wglub[:, ft, fo * 128:(fo + 1) * 128], yT[:, ft, tb * 512:(tb + 1) * 512], ft == 0, ft == 3,
                       ["wglub", "yT"], ["ps%d" % bank])
                act(sgt[:, fo, :], ps(bank), AF.Sigmoid, ["ps%d" % bank, "bglu_c"], ["sgt"], bias=bglu_c[:, fo:fo + 1])
            tt("dve", yT[:, 0:4, tb * 512:(tb + 1) * 512], yT[:, 0:4, tb * 512:(tb + 1) * 512], sgt, ALU.mult, ["yT", "sgt"], ["yT"])

        P.barrier()
        for h in range(4):
            for tb in range(NTB):
                bank = tb % 2
                for k in range(5):
                    mm(ps(bank), dgc[:, h, k, :], xmT[:, h, tb * 512 + k:tb * 512 + k + 512], k == 0, k == 4,
                       ["dgc", "xmT"], ["ps%d" % bank])
                act(xc[:, tb * 512:(tb + 1) * 512], ps(bank), AF.Silu, ["ps%d" % bank, "convb_c"], ["xc"], bias=convb_c[:, h:h + 1])
            ts("dve", xcs, xc, skip_c[:, h:h + 1], None, ALU.mult, None, ["xc", "skip_c"], ["xcs"])
            for tb in range(NTB):
                cs = slice(tb * 512, (tb + 1) * 512)
                mm(ps(0), wqb[:, h, :], xc[:, cs], True, True, ["wqb", "xc"], ["ps0"])
                cp("act", qT[:, cs], ps(0), ["ps0"], ["qT"])
                mm(ps(1), wkb[:, h, :], xc[:, cs], True, True, ["wkb", "xc"], ["ps1"])
                cp("dve", kT[:, cs], ps(1), ["ps1"], ["kT"])
            for r in range(2):
                cp("dve", esc[:, :, r], e2[:, :, r * 4 + h], ["e2"], ["esc"])
                cp("dve", esc[:, :, 2 + r], e3[:, :, r * 4 + h], ["e3"], ["esc"])
            for i in range(NT):
                bank = i % 2
                ts_ = slice(i * 128, (i + 1) * 128)
                mm(ps(bank)[:, 0:128], xc[:, ts_], wkb[:, h, :], True, True, ["wkb", "xc"], ["ps%d" % bank])
                mm(ps(bank)[:, 128:256], xmT[:, h, 2 + i * 128:2 + (i + 1) * 128], wvb[:, h, :], True, True, ["wvb", "xmT"], ["ps%d" % bank])
                cp("act", ktok[:, i, :], ps(bank)[:, 0:128], ["ps%d" % bank], ["ktok"])
                tt("dve", vtall[:, i, :, 0:128], ps(bank)[:, 128:256][:, None, :].broadcast_to([128, 4, 128]),
                   esc[:, i, :][:, :, None].broadcast_to([128, 4, 128]), ALU.mult, ["ps%d" % bank, "esc"], ["vtall"])
            cp("dve", vtall[:, :, :, 128], esc, ["esc"], ["vtall"])
            def rec_info(cc):
                info = []
                for r in range(2):
                    c = cc if r == 0 else NT - 1 - cc
                    bS = (6, 7)[cc % 2] if r == 0 else (2, 3)[cc % 2]
                    info.append((r, c, r * 4 + h, slice(c * 128, (c + 1) * 128), bS))
                return info

            def rec_front(cc):
                for (r, c, rh, cs, bS) in rec_info(cc):
                    mm(ps(bS)[:, 0:128], kT[:, cs], qT[:, cs], True, True, ["kT", "qT"], ["ps%d" % bS])
                    if cc < NT - 1:
                        mm(ps(bS)[:, 260:389], ktok[:, c, :], vtall[:, c, 2 + r, :], True, True, ["ktok", "vtall"], ["ps%d" % bS])

            rec_front(0)
            for cc in range(NT):
                info = rec_info(cc)
                if cc + 1 < NT:
                    rec_front(cc + 1)
                for (r, c, rh, cs, bS) in info:
                    if cc < NT - 1:
                        pc = ps(bS)[:, 260:389]
                        if cc == 0:
                            cp("dve", Cf[:, r, :], pc, ["ps%d" % bS], ["Cf%d" % r])
                        else:
                            stt("dve", Cf[:, r, :], Cf[:, r, :], eBL[:, c, rh:rh + 1], pc, ALU.mult, ALU.add,
                                ["Cf%d" % r, "eBL", "ps%d" % bS], ["Cf%d" % r])
                        cp("act", Cb[:, r, (cc + 1) % 2, :], Cf[:, r, :], ["Cf%d" % r], ["Cb%d%d" % (r, (cc + 1) % 2)])
                    msk = msk_f if r == 0 else msk_b
                    tt("dve", Sm[:, r, :], ps(bS)[:, 0:128], msk, ALU.mult, ["ps%d" % bS, "msk_f", "msk_b"], ["Sm%d" % r])
                for (r, c, rh, cs, bS) in info:
                    pn = ps(bS)[:, 128:257]
                    mm(pn, Sm[:, r, :], vtall[:, c, r, :], True, cc == 0, ["Sm%d" % r, "vtall"], ["ps%d" % bS])
                    if cc > 0:
                        mm(pn, qT[:, cs], Cb[:, r, cc % 2, :], False, True, ["qT", "Cb%d%d" % (r, cc % 2)], ["ps%d" % bS])
                    cp("act", nd[:, r, c, :], pn, ["ps%d" % bS], ["nd%d" % r])
            for r in range(2):
                rh = r * 4 + h
                den = nd[:, r, :, 128]
                e1r = e1[:, :, rh]
                dA, dB, dC = dnv[:, r, 0, :], dnv[:, r, 1, :], dnv[:, r, 2, :]
                tt("dve", dA, den, e1r, ALU.mult, ["nd%d" % r, "e1"], ["dnv%d" % r])
                ts("dve", dB, dA, -1.0, None, ALU.mult, None, ["dnv%d" % r], ["dnv%d" % r])
                tt("dve", dA, dA, dB, ALU.max, ["dnv%d" % r], ["dnv%d" % r])
                ts("dve", dA, dA, 1.0, None, ALU.max, None, ["dnv%d" % r], ["dnv%d" % r])
                V("dve", lambda e, dA=dA: e.reciprocal(out=dA, in_=dA), ["dnv%d" % r], ["dnv%d" % r])
                tt("dve", dC, dA, e1r, ALU.mult, ["dnv%d" % r, "e1"], ["dnv%d" % r])
            sc0 = dnv[:, 0, 2, :][:, :, None].broadcast_to([128, NT, 128])
            sc1 = dnv[:, 1, 2, :][:, :, None].broadcast_to([128, NT, 128])
            tt("dve", nd[:, 1, :, 0:128], nd[:, 1, :, 0:128], sc1, ALU.mult, ["nd1", "dnv1"], ["nd1"])
            tt("dve", hacc, nd[:, 0, :, 0:128], sc0, ALU.mult, ["nd0", "dnv0"], ["cacc"])
            tt("dve", hacc, hacc, nd[:, 1, :, 0:128], ALU.add, ["cacc", "nd1"], ["cacc"])
            V("dve", lambda e: e.tensor_reduce(out=lnv2[:, 0, :], in_=hacc, axis=mybir.AxisListType.X, op=ALU.add),
              ["cacc"], ["lnv2a"])
            for i in range(NT):
                act(hnb_all[:, i, :], hacc[:, i, :], AF.Square, ["cacc"], ["hnb_all", "lnv2b"], accum=lnv2[:, 1, i:i + 1])
            ts("dve", lnv2[:, 0, :], lnv2[:, 0, :], -1.0 / 128, None, ALU.mult, None, ["lnv2a"], ["lnv2a"])
            tt("dve", lnv3[:, 0, :], lnv2[:, 0, :], lnv2[:, 0, :], ALU.mult, ["lnv2a"], ["lnv3a"])
            stt("dve", lnv2[:, 1, :], lnv2[:, 1, :], 1.0 / 128, lnv3[:, 0, :], ALU.mult, ALU.subtract, ["lnv2b", "lnv3a"], ["lnv2b"])
            ts("dve", lnv2[:, 1, :], lnv2[:, 1, :], EPS, None, ALU.add, None, ["lnv2b"], ["lnv2b"])
            act(lnv2[:, 1, :], lnv2[:, 1, :], AF.Sqrt, ["lnv2b"], ["lnv2b"])
            V("dve", lambda e: e.reciprocal(out=lnv2[:, 1, :], in_=lnv2[:, 1, :]), ["lnv2b"], ["lnv2b"])
            tt("dve", lnv3[:, 1, :], lnv2[:, 0, :], lnv2[:, 1, :], ALU.mult, ["lnv2a", "lnv2b"], ["lnv3b"])
            for i in range(NT):
                act(hnb_all[:, i, :], hacc[:, i, :], AF.Identity, ["cacc", "lnv2b", "lnv3b"], ["hnb_all"],
                    scale=lnv2[:, 1, i:i + 1], bias=lnv3[:, 1, i:i + 1])
            GT = min(NT, 8)
            for jb in range(NT // GT):
                bank = 4 + jb % 2
                pb = psbf(bank)
                for ii in range(GT):
                    i = jb * GT + ii
                    tp(pb[:, ii * 128:(ii + 1) * 128], hnb_all[:, i, :], ident_b, ["hnb_all", "ident_b"], ["ps%d" % bank])
                cols = slice(jb * GT * 128, (jb + 1) * GT * 128)
                w = GT * 128
                stt("dve", otmp[:, 0:w], pb[:, 0:w], hnorm_c[:, h:h + 1], xcs[:, cols], ALU.mult, ALU.add,
                    ["ps%d" % bank, "hnorm_c", "xcs"], ["otmp"])
                tt("pool", yT[:, 4 + h, cols], otmp[:, 0:w], sigoT[:, h, cols], ALU.mult, ["otmp", "sigoT"], ["yT"])

        P.barrier()
        dma_in("woutl", wslot[:, :, 0:D], woutb_d.rearrange("c p n -> p c n"), ["wslot"], reads=["wscr1"])
        for i in range(NT):
            ts_ = slice(i * 128, (i + 1) * 128)
            sl = i % 2
            x1t, ytmp, sq3 = x1t2[:, sl, :], ytmp2[:, sl, :], ssqO[:, sl, :]
            kx1, kyt, ksq = "x1t%d" % sl, "ytmp%d" % sl, "ssqO%d" % sl
            dma_in("x%d" % sl, xt[:, sl, :], x[t0 + i * 128:t0 + (i + 1) * 128, :], ["xt%d" % sl])
            bks = (0, 1) if sl == 0 else (2, 3)
            for half in range(2):
                bk = bks[half]
                for ft in range(8):
                    mm(ps(bk), yT[:, ft, ts_], wslot[:, ft, half * 512:(half + 1) * 512], ft == 0, ft == 7,
                       ["yT", "wslot"], ["ps%d" % bk])
                act(ytmp[:, half * 512:(half + 1) * 512], ps(bk), AF.Square, ["ps%d" % bk], [kyt, ksq], accum=sq3[:, 1 + half:2 + half])
            tt("dve", sq3[:, 1:2], sq3[:, 1:2], sq3[:, 2:3], ALU.add, [ksq], [ksq])
            rstd_from_ss(sq3[:, 1:2], D, ksq)
            for half in range(2):
                hs = slice(half * 512, (half + 1) * 512)
                stt("dve", ytmp[:, hs], ps(bks[half]), sq3[:, 1:2], gpost[:, hs], ALU.mult, ALU.mult, ["ps%d" % bks[half], ksq, "gpost"], [kyt])
            tt("pool", x1t[:, 0:512], ytmp[:, 0:512], xt[:, sl, 0:512], ALU.add, [kyt, "xt%d" % sl], [kx1])
            tt("dve", x1t[:, 512:1024], ytmp[:, 512:1024], xt[:, sl, 512:1024], ALU.add, [kyt, "xt%d" % sl], [kx1 + "b"])
            P.dma("sp", "x1w%d" % sl, lambda e, i=i, t0=t0, x1t=x1t: e.dma_start(out=x1_d[t0 + i * 128:t0 + (i + 1) * 128, :], in_=x1t),
                  reads=[kx1, kx1 + "b"], writes=["x1_d"])
        P.barrier()

    P.barrier()
    AR.off = persist_end
    stgB = AR.alloc([2, 1408], F32)
    wgb = AR.alloc([8, D_FF], BF)
    wub = AR.alloc([8, D_FF], BF)
    wdb = AR.alloc([NFF, D], BF)
    x1b = AR.alloc([4, D], F32)
    xnb2 = AR.alloc([D], BF)
    ssq2 = AR.alloc([4], F32)
    ssqF2 = AR.alloc([4], F32)
    h2T = AR.alloc([8, 512], BF)
    sgf = AR.alloc([2, 512], BF)
    actT = AR.alloc([NFF, 512], BF)
    otile_a = AR.alloc([D], F32)
    otile_b = stgB[:, 0, 0:D]
    otiles = (otile_a, otile_b)

    def load_convert_b(src_rows, ncols, dst, scale_col, keyw):
        for i, src in enumerate(src_rows):
            sl = i % 2
            sap = stgB[:, sl, 0:ncols]
            dma_in("stgB%d" % sl, sap, src, ["stgB%d" % sl])
            d_ap = dst(i)
            if scale_col is not None:
                sc = scale_col(i)
                if i % 2 == 0:
                    V("act", lambda e, d_ap=d_ap, sap=sap, sc=sc: e.activation(out=d_ap, in_=sap, func=AF.Copy, scale=sc),
                      ["stgB%d" % sl, "gfpre_c"], [keyw])
                else:
                    V("dve", lambda e, d_ap=d_ap, sap=sap, sc=sc: e.tensor_scalar(out=d_ap, in0=sap, scalar1=sc, scalar2=None, op0=ALU.mult),
                      ["stgB%d" % sl, "gfpre_c"], [keyw])
            else:
                if i % 2 == 0:
                    V("act", lambda e, d_ap=d_ap, sap=sap: e.activation(out=d_ap, in_=sap, func=AF.Copy), ["stgB%d" % sl], [keyw])
                else:
                    V("dve", lambda e, d_ap=d_ap, sap=sap: e.tensor_copy(out=d_ap, in_=sap), ["stgB%d" % sl], [keyw])

    HF = D_FF // 2
    load_convert_b([w_gate[(i // 2) * 128:(i // 2 + 1) * 128, (i % 2) * HF:(i % 2 + 1) * HF] for i in range(16)], HF,
                   lambda i: wgb[:, i // 2, (i % 2) * HF:(i % 2 + 1) * HF], lambda i: gfpre_c[:, i // 2:i // 2 + 1], "wgb")
    load_convert_b([w_up[(i // 2) * 128:(i // 2 + 1) * 128, (i % 2) * HF:(i % 2 + 1) * HF] for i in range(16)], HF,
                   lambda i: wub[:, i // 2, (i % 2) * HF:(i % 2 + 1) * HF], lambda i: gfpre_c[:, i // 2:i // 2 + 1], "wub")
    load_convert_b([w_down[c * 128:(c + 1) * 128, :] for c in range(NFF)], D, lambda i: wdb[:, i, :], None, "wdb")

    def norm_transpose_b(src_ap, src_key, col0, bank):
        act(xnb2, src_ap, AF.Square, [src_key], ["xnb2", "ssq2"], accum=ssq2[:, 0:1])
        act(ssq2[:, 0:1], ssq2[:, 0:1], AF.Sqrt, ["ssq2", "eps_c"], ["ssq2"], scale=1.0 / D, bias=eps_c)
        V("dve", lambda e: e.reciprocal(out=ssq2[:, 0:1], in_=ssq2[:, 0:1]), ["ssq2"], ["ssq2"])
        act(xnb2, src_ap, AF.Copy, [src_key, "ssq2"], ["xnb2"], scale=ssq2[:, 0:1])
        pb = psbf(bank)
        for c in range(8):
            tp(pb[:, c * 128:(c + 1) * 128], xnb2[:, c * 128:(c + 1) * 128], ident_b, ["xnb2", "ident_b"], ["ps%d" % bank])
        cp("dve", h2T[:, :, col0:col0 + 128], pb.rearrange("p (c t) -> p c t", t=128), ["ps%d" % bank], ["h2T"])

    P.barrier()
    ocnt = 0
    xin = stgB[:, 1, 0:D]
    NBLK = NTOK // 512

    def prep_block(blk):
        for j in range(4):
            r0 = blk * 512 + j * 128
            dma_in("xinl", xin, x1_d[r0:r0 + 128, :], ["xin"], reads=["x1_d"])
            norm_transpose_b(xin, "xin", j * 128, j % 2)

    prep_block(0)
    for blk in range(NBLK):
        for j in range(4):
            r0 = blk * 512 + j * 128
            dma_in("x1l%d" % j, x1b[:, j, :], x1_d[r0:r0 + 128, :], ["x1b%d" % j], reads=["x1_d"])
        for f in range(NFF):
            bg, bu = (0, 1) if f % 2 == 0 else (2, 3)
            for c in range(8):
                mm(ps(bg), wgb[:, c, f * 128:(f + 1) * 128], h2T[:, c, :], c == 0, c == 7, ["wgb", "h2T"], ["ps%d" % bg])
            for c in range(8):
                mm(ps(bu), wub[:, c, f * 128:(f + 1) * 128], h2T[:, c, :], c == 0, c == 7, ["wub", "h2T"], ["ps%d" % bu])
            act(sgf[:, f % 2, :], ps(bg), AF.Silu, ["ps%d" % bg], ["sgf%d" % (f % 2)])
            tt("dve", actT[:, f, :], sgf[:, f % 2, :], ps(bu), ALU.mult, ["sgf%d" % (f % 2), "ps%d" % bu], ["actT"])
        if blk + 1 < NBLK:
            prep_block(blk + 1)
        for j in range(4):
            r0 = blk * 512 + j * 128
            osl = ocnt % 2
            ocnt += 1
            otile = otiles[osl]
            bks = (4, 5) if osl == 0 else (6, 7)
            ksq = "ssqF%d" % osl
            sqF = ssq2[:, 0:4] if osl == 0 else ssqF2[:, 0:4]
            for half in range(2):
                bank = bks[half]
                for f in range(NFF):
                    mm(ps(bank), actT[:, f, j * 128:(j + 1) * 128], wdb[:, f, half * 512:(half + 1) * 512], f == 0, f == NFF - 1,
                       ["actT", "wdb"], ["ps%d" % bank])
                act(otile[:, half * 512:(half + 1) * 512], ps(bank), AF.Square, ["ps%d" % bank], ["otile%d" % osl, ksq],
                    accum=sqF[:, 1 + half:2 + half])
            tt("dve", sqF[:, 1:2], sqF[:, 1:2], sqF[:, 2:3], ALU.add, [ksq], [ksq])
            act(sqF[:, 1:2], sqF[:, 1:2], AF.Sqrt, [ksq, "eps_c"], [ksq], scale=1.0 / D, bias=eps_c)
            V("dve", lambda e, sqF=sqF: e.reciprocal(out=sqF[:, 1:2], in_=sqF[:, 1:2]), [ksq], [ksq])
            for half in range(2):
                hs = slice(half * 512, (half + 1) * 512)
                stt("dve", otile[:, hs], ps(bks[half]), sqF[:, 1:2], gpost2[:, hs], ALU.mult, ALU.mult,
                    ["ps%d" % bks[half], ksq, "gpost2"], ["otile%d" % osl])
            tt("pool", otile[:, 0:512], otile[:, 0:512], x1b[:, j, 0:512], ALU.add, ["otile%d" % osl, "x1b%d" % j], ["otileA%d" % osl])
            tt("dve", otile[:, 512:1024], otile[:, 512:1024], x1b[:, j, 512:1024], ALU.add, ["otile%d" % osl, "x1b%d" % j], ["otileB%d" % osl])
            P.dma("sp", "ow%d" % osl, lambda e, r0=r0, otile=otile: e.dma_start(out=out[r0:r0 + 128, :], in_=otile),
                  reads=["otile%d" % osl, "otileA%d" % osl, "otileB%d" % osl], writes=["out%d" % osl])
    P.fence("sp", ["out0", "out1"])
    P.emit(st)
    st.close()
    return nc


INPUT_ORDER = ["x", "norm_mix_pre", "norm_mix_post", "norm_ffn_pre", "norm_ffn_post", "w_in", "ml_gate_bias",
               "s5_lam_re", "s5_lam_im", "s5_log_dt", "s5_b_re", "s5_b_im", "s5_c_re", "s5_c_im", "s5_d",
               "s5_w_glu", "s5_b_glu", "ml_conv_w", "ml_conv_b", "ml_wq", "ml_wk", "ml_wv", "ml_head_norm",
               "ml_skip", "w_out", "w_gate", "w_up", "w_down"]

_SHAPES = {"norm_mix_pre": [D], "norm_mix_post": [D], "norm_ffn_pre": [D], "norm_ffn_post": [D],
           "w_in": [D, D_IN], "ml_gate_bias": [16], "s5_lam_re": [64, 64], "s5_lam_im": [64, 64],
           "s5_log_dt": [64], "s5_b_re": [32, 64, 16], "s5_b_im": [32, 64, 16], "s5_c_re": [512, 64],
           "s5_c_im": [512, 64], "s5_d": [512], "s5_w_glu": [512, 512], "s5_b_glu": [512],
           "ml_conv_w": [5, 512], "ml_conv_b": [512], "ml_wq": [4, 128, 128], "ml_wk": [4, 128, 128],
           "ml_wv": [4, 128, 128], "ml_head_norm": [512], "ml_skip": [512], "w_out": [D, D],
           "w_gate": [D, D_FF], "w_up": [D, D_FF], "w_down": [D_FF, D]}


def make_in_map(inputs, xs):
    m = {"x": np.ascontiguousarray(xs, dtype=np.float32)}
    for k, shp in _SHAPES.items():
        m[k] = np.ascontiguousarray(np.asarray(inputs[k], dtype=np.float32).reshape(shp))
    return m


def kernel(**inputs):
    x = np.asarray(inputs["x"], dtype=np.float32)
    B, S, _ = x.shape
    ncores = 8
    nseq = B // ncores
    nc = build_program(nseq, S)
    in_maps = []
    for i in range(ncores):
        xs = x[i * nseq:(i + 1) * nseq].reshape(nseq * S, D)
        in_maps.append(make_in_map(inputs, xs))
    res = run_bass_kernel_spmd(nc, in_maps, core_ids=list(range(ncores)))
    outs = [np.asarray(r["out"], dtype=np.float32).reshape(nseq, S, D) for r in res.results]
    return np.concatenate(outs, axis=0)
```

```python
import math
from contextlib import ExitStack

import numpy as np
import concourse.bass as bass
import concourse.mybir as mybir
from concourse.bass_utils import run_bass_kernel_spmd

F32 = mybir.dt.float32
BF = mybir.dt.bfloat16
I32 = mybir.dt.int32
AF = mybir.ActivationFunctionType
ALU = mybir.AluOpType

D = 1024
D_S5 = 512
D_IN = 1552
D_FF = 2816
NFF = D_FF // 128
EPS = 1e-6
TWO_PI = 2.0 * math.pi


class Prog:
    COMPUTE = ("pe", "act", "dve", "pool")

    def __init__(self, nc, same_engine_sync=True):
        self.nc = nc
        self.ops = []
        self.same_engine_sync = same_engine_sync

    def op(self, eng, fn, reads=(), writes=()):
        ps_r = tuple(r for r in reads if isinstance(r, str) and r.startswith("ps") and r[2:].isdigit())
        if ps_r:
            reads = tuple(r for r in reads if r not in ps_r)
            writes = tuple(writes) + tuple(r for r in ps_r if r not in writes)
        self.ops.append(dict(kind="c", eng=eng, fn=fn, reads=tuple(reads), writes=tuple(writes)))

    def dma(self, queue, stream, fn, reads=(), writes=()):
        self.ops.append(dict(kind="d", eng=queue, stream=stream, fn=fn,
                             reads=tuple(reads), writes=tuple(writes)))

    def fence(self, eng, reads):
        self.ops.append(dict(kind="f", eng=eng, fn=None, reads=tuple(reads), writes=()))

    def barrier(self):
        for e in ("pe", "act", "dve", "pool", "sp"):
            self.ops.append(dict(kind="b", eng=e, fn=None, reads=(), writes=()))

    def emit(self, stack):
        nc = self.nc
        ops = self.ops
        last_w = {}
        readers = {}
        deps = []
        needed = set()
        last_of = {}
        for i, o in enumerate(ops):
            d = set()
            if o["kind"] == "b":
                d = set(last_of.values())
            for r in o["reads"]:
                if r in last_w:
                    d.add(last_w[r])
            for w in o["writes"]:
                if w in last_w:
                    d.add(last_w[w])
                for rr in readers.get(w, ()):
                    d.add(rr)
            d.discard(i)
            for r in o["reads"]:
                readers.setdefault(r, []).append(i)
            for w in o["writes"]:
                last_w[w] = i
                readers[w] = []
            if o["kind"] == "c":
                last_of[o["eng"]] = i
            elif o["kind"] == "d":
                last_of[("d", o["stream"])] = i
            dd = set()
            for j in d:
                oj = ops[j]
                if oj["kind"] in ("f", "b"):
                    continue
                if oj["kind"] == "c" and o["kind"] == "c" and oj["eng"] == o["eng"]:
                    if o["eng"] == "pe" or not self.same_engine_sync:
                        continue
                if oj["kind"] == "c" and o["kind"] == "b" and oj["eng"] == o["eng"]:
                    continue
                dd.add(j)
            deps.append(dd)
            needed |= dd
        eng_sem = {}
        for e in self.COMPUTE:
            eng_sem[e] = stack.enter_context(nc.semaphore("sem_" + e))
        stream_sem = {}
        counts = {}
        sig = {}
        for i, o in enumerate(ops):
            if o["kind"] == "c":
                if i in needed:
                    counts[o["eng"]] = counts.get(o["eng"], 0) + 1
                    sig[i] = (o["eng"], counts[o["eng"]])
            elif o["kind"] == "d":
                s = o["stream"]
                if s not in stream_sem:
                    stream_sem[s] = stack.enter_context(nc.semaphore("sd_%d" % len(stream_sem)))
                counts[("d", s)] = counts.get(("d", s), 0) + 16
                sig[i] = (("d", s), counts[("d", s)])
        self.counts = counts

        def semof(k):
            return eng_sem[k] if not isinstance(k, tuple) else stream_sem[k[1]]

        per_eng = {}
        for i, o in enumerate(ops):
            per_eng.setdefault(o["eng"], []).append(i)

        block = stack.enter_context(nc.Block())
        handles = {"pe": block.tensor, "act": block.scalar, "dve": block.vector,
                   "pool": block.gpsimd, "sp": block.sync}

        def make(idxs):
            def body(eng):
                waited = {}
                for i in idxs:
                    o = ops[i]
                    need = {}
                    for j in deps[i]:
                        k, v = sig[j]
                        if need.get(k, 0) < v:
                            need[k] = v
                    for k, v in need.items():
                        if waited.get(k, 0) >= v:
                            continue
                        eng.wait_ge(semof(k), v)
                        waited[k] = v
                    if o["kind"] in ("f", "b"):
                        continue
                    ins = o["fn"](eng)
                    if i in sig:
                        k, v = sig[i]
                        ins.then_inc(semof(k), 16 if isinstance(k, tuple) else 1)
            return body

        for engname, idxs in per_eng.items():
            handles[engname](make(idxs))


class Arena:
    def __init__(self, base_ap, nwords):
        self.base = base_ap
        self.n = nwords
        self.off = 0

    def alloc(self, shape, dtype):
        nel = 1
        for s in shape:
            nel *= s
        nbytes = nel * (4 if dtype in (F32, I32) else 2)
        nw = (nbytes + 3) // 4
        nw = (nw + 7) // 8 * 8
        assert self.off + nw <= self.n, ("arena overflow", self.off, nw, self.n)
        ap = self.base[:, self.off:self.off + nw]
        self.off += nw
        if dtype != F32:
            ap = ap.bitcast(dtype)
        ap = ap[:, 0:nel]
        if len(shape) == 2:
            ap = ap.rearrange("p (a b) -> p a b", b=shape[1])
        elif len(shape) == 3:
            ap = ap.rearrange("p (a b c) -> p a b c", b=shape[1], c=shape[2])
        elif len(shape) == 4:
            ap = ap.rearrange("p (a b c d) -> p a b c d", b=shape[1], c=shape[2], d=shape[3])
        return ap


def build_program(nseq, S, taps=()):
    assert S % 1024 == 0
    NT = S // 128
    NCH = S // 8
    NHB = S // 1024
    NTB = S // 512
    NTOK = nseq * S

    nc = bass.Bass("TRN2", target_bir_lowering=False)
    dt_in = {}

    def din(name, shape):
        dt_in[name] = nc.dram_tensor(name, list(shape), F32, kind="ExternalInput").ap()
        return dt_in[name]

    x = din("x", [NTOK, D])
    g_mix_pre = din("norm_mix_pre", [D])
    g_mix_post = din("norm_mix_post", [D])
    g_ffn_pre = din("norm_ffn_pre", [D])
    g_ffn_post = din("norm_ffn_post", [D])
    w_in = din("w_in", [D, D_IN])
    gate_bias = din("ml_gate_bias", [16])
    lam_re = din("s5_lam_re", [64, 64])
    lam_im = din("s5_lam_im", [64, 64])
    log_dt = din("s5_log_dt", [64])
    b_re = din("s5_b_re", [32, 64, 16])
    b_im = din("s5_b_im", [32, 64, 16])
    c_re = din("s5_c_re", [512, 64])
    c_im = din("s5_c_im", [512, 64])
    s5_d = din("s5_d", [512])
    w_glu = din("s5_w_glu", [512, 512])
    b_glu = din("s5_b_glu", [512])
    conv_w = din("ml_conv_w", [5, 512])
    conv_b = din("ml_conv_b", [512])
    wq = din("ml_wq", [4, 128, 128])
    wk = din("ml_wk", [4, 128, 128])
    wv = din("ml_wv", [4, 128, 128])
    head_norm = din("ml_head_norm", [512])
    skip = din("ml_skip", [512])
    w_out = din("w_out", [D, D])
    w_gate = din("w_gate", [D, D_FF])
    w_up = din("w_up", [D, D_FF])
    w_down = din("w_down", [D_FF, D])
    out = nc.dram_tensor("out", [NTOK, D], F32, kind="ExternalOutput").ap()
    x1_d = nc.dram_tensor("x1_scr", [NTOK, D], F32, kind="Internal").ap()
    s5m_d = nc.dram_tensor("s5m_scr", [32, 128, 640], BF, kind="Internal").ap()
    winb_d = nc.dram_tensor("winb_scr", [8, 128, D_IN], BF, kind="Internal").ap()
    woutb_d = nc.dram_tensor("woutb_scr", [8, 128, D], BF, kind="Internal").ap()
    tap_out = {}
    for tname, tshape in taps:
        tap_out[tname] = nc.dram_tensor("tap_" + tname, list(tshape), F32, kind="ExternalOutput").ap()

    st = ExitStack()
    AW = 53200
    arena_t = st.enter_context(nc.sbuf_tensor("arena", [128, AW], F32))
    AR = Arena(arena_t[:], AW)
    psb = [st.enter_context(nc.psum_tensor("ps%d" % i, [128, 512], F32)) for i in range(8)]
    P = Prog(nc)

    def ps(i):
        return psb[i][:]

    def psbf(i):
        return psb[i][:].bitcast(BF)

    ident_b = AR.alloc([128], BF)
    gpost2 = AR.alloc([D], F32)
    gfpre_c = AR.alloc([8], F32)
    eps_c = AR.alloc([1], F32)
    persist_end = AR.off
    ident_f = AR.alloc([128], F32)
    ones_f = AR.alloc([128], F32)
    tri_f = AR.alloc([128], F32)
    tri_b = AR.alloc([128], F32)
    msk_f = AR.alloc([128], BF)
    msk_b = AR.alloc([128], BF)
    gpost = AR.alloc([D], F32)
    gbias = AR.alloc([16], F32)
    gpre_c = AR.alloc([8], F32)
    bglu_c = AR.alloc([4], F32)
    convw_c = AR.alloc([4, 5], F32)
    convb_c = AR.alloc([4], F32)
    hnorm_c = AR.alloc([4], F32)
    skip_c = AR.alloc([4], F32)
    one_c = AR.alloc([1], F32)
    coefA = AR.alloc([32], F32)
    coefB = AR.alloc([2, 32], F32)
    wglub = AR.alloc([4, 512], BF)
    wqb = AR.alloc([4, 128], BF)
    wkb = AR.alloc([4, 128], BF)
    wvb = AR.alloc([4, 128], BF)
    dgc = AR.alloc([4, 5, 128], BF)

    def V(eng, fn, reads, writes):
        P.op(eng, fn, reads, writes)

    def dma_in(stream, dst, src, writes, reads=(), slow=False):
        P.dma("sp", stream, lambda e: e.dma_start(out=dst, in_=src, allow_slow_non_contiguous=slow),
              reads=reads, writes=writes)

    V("pool", lambda e: e.memset(ones_f, 1.0), [], ["ones_f"])
    V("pool", lambda e: e.memset(eps_c, EPS), [], ["eps_c"])
    V("pool", lambda e: e.memset(one_c, 1.0), [], ["one_c"])
    V("pool", lambda e: e.affine_select(out=ident_f, in_=ones_f, pattern=[[-1, 128]], compare_op=ALU.is_equal,
                                        fill=0.0, base=0, channel_multiplier=1), ["ones_f"], ["ident_f"])
    V("pool", lambda e: e.affine_select(out=tri_f, in_=ones_f, pattern=[[1, 128]], compare_op=ALU.is_ge,
                                        fill=0.0, base=0, channel_multiplier=-1), ["ones_f"], ["tri_f"])
    V("pool", lambda e: e.affine_select(out=tri_b, in_=ones_f, pattern=[[-1, 128]], compare_op=ALU.is_ge,
                                        fill=0.0, base=0, channel_multiplier=1), ["ones_f"], ["tri_b"])
    V("dve", lambda e: e.tensor_copy(out=ident_b, in_=ident_f), ["ident_f"], ["ident_b"])
    V("dve", lambda e: e.tensor_copy(out=msk_f, in_=tri_f), ["tri_f"], ["msk_f"])
    V("dve", lambda e: e.tensor_copy(out=msk_b, in_=tri_b), ["tri_b"], ["msk_b"])
    dma_in("c0", gpost, g_mix_post.partition_broadcast(128), ["gpost"])
    dma_in("c1", gpost2, g_ffn_post.partition_broadcast(128), ["gpost2"])
    dma_in("c2", gbias, gate_bias.partition_broadcast(128), ["gbias"])
    dma_in("c3", gpre_c, g_mix_pre.rearrange("(c p) -> p c", p=128), ["gpre_c"], slow=True)
    dma_in("c4", gfpre_c, g_ffn_pre.rearrange("(c p) -> p c", p=128), ["gfpre_c"], slow=True)
    dma_in("c5", bglu_c, b_glu.rearrange("(c p) -> p c", p=128), ["bglu_c"], slow=True)
    dma_in("c6", convb_c, conv_b.rearrange("(c p) -> p c", p=128), ["convb_c"], slow=True)
    dma_in("c7", hnorm_c, head_norm.rearrange("(c p) -> p c", p=128), ["hnorm_c"], slow=True)
    dma_in("c8", skip_c, skip.rearrange("(c p) -> p c", p=128), ["skip_c"], slow=True)
    for k in range(5):
        dma_in("c9", convw_c[:, :, k], conv_w[k].rearrange("(c p) -> p c", p=128), ["convw_c%d" % k], slow=True)
    CONVW = ["convw_c%d" % k for k in range(5)]

    stg = AR.alloc([2, D_IN], F32)

    def load_convert(src_rows, ncols, dst, scale_col, tag, keyw, eng_cycle=("act", "dve")):
        for i, src in enumerate(src_rows):
            sl = i % 2
            sap = stg[:, sl, 0:ncols]
            dma_in("stg%d" % sl, sap, src, ["stg%d" % sl])
            d_ap = dst(i)
            sc = scale_col(i) if scale_col is not None else None
            en = eng_cycle[i % len(eng_cycle)]
            if en == "act":
                if sc is None:
                    V("act", lambda e, d_ap=d_ap, sap=sap: e.activation(out=d_ap, in_=sap, func=AF.Copy),
                      ["stg%d" % sl], [keyw])
                else:
                    V("act", lambda e, d_ap=d_ap, sap=sap, sc=sc: e.activation(out=d_ap, in_=sap, func=AF.Copy, scale=sc),
                      ["stg%d" % sl, "gpre_c", "gfpre_c"], [keyw])
            else:
                if sc is None:
                    V("dve", lambda e, d_ap=d_ap, sap=sap: e.tensor_copy(out=d_ap, in_=sap), ["stg%d" % sl], [keyw])
                else:
                    V("dve", lambda e, d_ap=d_ap, sap=sap, sc=sc: e.tensor_scalar(out=d_ap, in0=sap, scalar1=sc, scalar2=None, op0=ALU.mult),
                      ["stg%d" % sl, "gpre_c", "gfpre_c"], [keyw])

    load_convert([w_glu[c * 128:(c + 1) * 128, :] for c in range(4)], 512, lambda i: wglub[:, i, :], None, "wglu", "wglub")
    load_convert([wq[h] for h in range(4)], 128, lambda i: wqb[:, i, :], None, "wq", "wqb")
    for h in range(4):
        sl = h % 2
        sap = stg[:, sl, 0:128]
        dma_in("stg%d" % sl, sap, wk[h], ["stg%d" % sl])
        V("act", lambda e, h=h, sap=sap: e.activation(out=wkb[:, h, :], in_=sap, func=AF.Copy, scale=float(128 ** -0.5)),
          ["stg%d" % sl], ["wkb"])
    load_convert([wv[h] for h in range(4)], 128, lambda i: wvb[:, i, :], None, "wv", "wvb")

    mark = AR.off
    cvb = AR.alloc([2, D_IN], BF)
    Lin = AR.alloc([2, 128], F32)
    LRt = AR.alloc([64], F32)
    LIt = AR.alloc([64], F32)
    DTt = AR.alloc([64], F32)
    lrd = AR.alloc([64], F32)
    ang = AR.alloc([64], F32)
    jv = AR.alloc([16], F32)
    Fm = AR.alloc([64, 16], F32)
    Fs = AR.alloc([64, 16], F32)
    Fc = AR.alloc([64, 16], F32)
    Fi = AR.alloc([64, 16], I32)
    Ff = AR.alloc([64, 16], F32)
    Fg = AR.alloc([64, 16], F32)
    Wre = AR.alloc([64, 16], F32)
    Wim = AR.alloc([64, 16], F32)
    t64a = AR.alloc([64], F32)
    t64b = AR.alloc([64], F32)
    t64c = AR.alloc([64], F32)
    fr = AR.alloc([64], F32)
    fi = AR.alloc([64], F32)
    Gre = AR.alloc([64, 8], F32)
    Gim = AR.alloc([64, 8], F32)
    Gt = AR.alloc([64, 8], F32)
    m0 = AR.alloc([1], F32)
    m1 = AR.alloc([1], F32)
    nm0 = AR.alloc([1], F32)
    nm1 = AR.alloc([1], F32)
    XA = AR.alloc([64, 8], F32)
    XB = AR.alloc([64, 8], F32)
    Bre_t = AR.alloc([32, 16], F32)
    Bim_t = AR.alloc([32, 16], F32)
    Cin = AR.alloc([4, 2, 128], F32)
    Cre_t = AR.alloc([32, 16], F32)
    Cim_t = AR.alloc([32, 16], F32)
    Dcol = AR.alloc([32], F32)
    bmf = AR.alloc([8, 16], F32)
    bmb = AR.alloc([8, 16], F32)
    Pst = AR.alloc([2, 16, 8, 16], F32)
    Hst = AR.alloc([2, 16, 8, 16], F32)
    Qre_s = AR.alloc([16, 8, 16], F32)
    Qim_s = AR.alloc([16, 8, 16], F32)
    Pre_s = AR.alloc([16, 8, 16], F32)
    Pim_s = AR.alloc([16, 8, 16], F32)
    tmpA = AR.alloc([16, 8, 16], F32)
    tmpB = AR.alloc([16, 8, 16], F32)
    s5o = AR.alloc([2, 5, 128], BF)
    RW = AR.alloc([4, 16, 8], F32)
    tT1 = AR.alloc([128], F32)
    tT2 = AR.alloc([128], F32)

    def tt(eng, o, a, b, op, reads, writes):
        V(eng, lambda e: e.tensor_tensor(out=o, in0=a, in1=b, op=op), reads, writes)

    def ts(eng, o, a, s1, s2, op0, op1, reads, writes):
        if op1 is None:
            V(eng, lambda e: e.tensor_scalar(out=o, in0=a, scalar1=s1, scalar2=None, op0=op0), reads, writes)
        else:
            V(eng, lambda e: e.tensor_scalar(out=o, in0=a, scalar1=s1, scalar2=s2, op0=op0, op1=op1), reads, writes)

    def act(o, a, func, reads, writes, scale=None, bias=None, accum=None, eng="act"):
        kw = {}
        if scale is not None:
            kw["scale"] = scale
        if bias is not None:
            kw["bias"] = bias
        if accum is not None:
            kw["accum_out"] = accum
        V(eng, lambda e: e.activation(out=o, in_=a, func=func, **kw), reads, writes)

    def stt(eng, o, a, s, b, op0, op1, reads, writes):
        V(eng, lambda e: e.scalar_tensor_tensor(out=o, in0=a, scalar=s, in1=b, op0=op0, op1=op1), reads, writes)

    def cp(eng, o, a, reads, writes):
        if eng == "act":
            V(eng, lambda e: e.activation(out=o, in_=a, func=AF.Copy), reads, writes)
        else:
            V(eng, lambda e: e.tensor_copy(out=o, in_=a), reads, writes)

    def mm(o, lhsT, rhs, start, stop, reads, writes):
        V("pe", lambda e: e.matmul(o, lhsT, rhs, start=start, stop=stop), reads, writes)

    def tp(o, in_, ident, reads, writes):
        V("pe", lambda e: e.transpose(o, in_, ident), reads, writes)

    for h in range(4):
        for k in range(5):
            ts("dve", dgc[:, h, k, :], ident_f, convw_c[:, h, k:k + 1], None, ALU.mult, None, ["ident_f"] + CONVW, ["dgc"])

    for ri, src in enumerate((lam_re, lam_im)):
        for dup in range(2):
            dma_in("pl%d%d" % (ri, dup), Lin[0:64, ri, dup * 64:(dup + 1) * 64], src, ["Lin"])
    dma_in("pl_dt", DTt, log_dt.partition_broadcast(128), ["DTt"])
    tp(ps(0)[:, 0:64], Lin[0:64, 0, :], ident_f[0:64, 0:64], ["Lin", "ident_f"], ["ps0"])
    tp(ps(0)[:, 64:128], Lin[0:64, 1, :], ident_f[0:64, 0:64], ["Lin", "ident_f"], ["ps0"])
    cp("dve", LRt, ps(0)[:, 0:64], ["ps0"], ["LRt"])
    cp("dve", LIt, ps(0)[:, 64:128], ["ps0"], ["LIt"])
    act(DTt, DTt, AF.Exp, ["DTt"], ["DTt"])
    tt("dve", lrd, LRt, DTt, ALU.mult, ["LRt", "DTt"], ["lrd"])
    tt("dve", ang, LIt, DTt, ALU.mult, ["LIt", "DTt"], ["ang"])
    ts("dve", ang, ang, 1.0 / TWO_PI, None, ALU.mult, None, ["ang"], ["ang"])
    for jj in range(16):
        V("pool", lambda e, jj=jj: e.memset(jv[:, jj:jj + 1], float(jj - 7)), [], ["jv"])
    V("pool", lambda e: e.memset(m0, 0.0), [], ["m0"])
    V("pool", lambda e: e.memset(m0[0:64, :], 1.0), ["m0"], ["m0"])
    V("pool", lambda e: e.memset(m1, 1.0), [], ["m1"])
    V("pool", lambda e: e.memset(m1[0:64, :], 0.0), ["m1"], ["m1"])
    ts("dve", nm0, m0, -1.0, None, ALU.mult, None, ["m0"], ["nm0"])
    ts("dve", nm1, m1, -1.0, None, ALU.mult, None, ["m1"], ["nm1"])
    jvb = jv[:, None, :].broadcast_to([128, 64, 16])
    tt("dve", Fm, lrd[:, :, None].broadcast_to([128, 64, 16]), jvb, ALU.mult, ["lrd", "jv"], ["Fm"])
    tt("dve", Fs, ang[:, :, None].broadcast_to([128, 64, 16]), jvb, ALU.mult, ["ang", "jv"], ["Fs"])
    ts("dve", Fc, Fs, 0.25, None, ALU.add, None, ["Fs"], ["Fc"])
    act(Fm, Fm, AF.Exp, ["Fm"], ["Fm"])

    def range_reduce(Fx, key):
        cp("dve", Fi, Fx, [key], ["Fi"])
        cp("dve", Ff, Fi, ["Fi"], ["Ff"])
        tt("dve", Fx, Fx, Ff, ALU.subtract, [key, "Ff"], [key])
        ts("dve", Fg, Fx, 0.5, None, ALU.is_gt, None, [key], ["Fg"])
        tt("dve", Fx, Fx, Fg, ALU.subtract, [key, "Fg"], [key])
        ts("dve", Fg, Fx, -0.5, None, ALU.is_lt, None, [key], ["Fg"])
        tt("dve", Fx, Fx, Fg, ALU.add, [key, "Fg"], [key])

    range_reduce(Fs, "Fs")
    range_reduce(Fc, "Fc")
    SIN_SCALE = TWO_PI * (1.0 - 2e-6)
    act(Fs, Fs, AF.Sin, ["Fs"], ["Fs"], scale=SIN_SCALE)
    act(Fc, Fc, AF.Sin, ["Fc"], ["Fc"], scale=SIN_SCALE)
    tt("dve", Wre, Fm, Fc, ALU.mult, ["Fm", "Fc"], ["Wre"])
    tt("dve", Wim, Fm, Fs, ALU.mult, ["Fm", "Fs"], ["Wim"])
    ts("dve", t64a, Wre[:, :, 8], -1.0, None, ALU.add, None, ["Wre"], ["t64a"])
    tt("dve", t64b, LRt, LRt, ALU.mult, ["LRt"], ["t64b"])
    tt("dve", t64c, LIt, LIt, ALU.mult, ["LIt"], ["t64c"])
    tt("dve", t64b, t64b, t64c, ALU.add, ["t64b", "t64c"], ["t64b"])
    V("dve", lambda e: e.reciprocal(out=t64b, in_=t64b), ["t64b"], ["t64b"])
    tt("dve", fr, t64a, LRt, ALU.mult, ["t64a", "LRt"], ["fr"])
    tt("dve", t64c, Wim[:, :, 8], LIt, ALU.mult, ["Wim", "LIt"], ["t64c"])
    tt("dve", fr, fr, t64c, ALU.add, ["fr", "t64c"], ["fr"])
    tt("dve", fr, fr, t64b, ALU.mult, ["fr", "t64b"], ["fr"])
    tt("dve", fi, Wim[:, :, 8], LRt, ALU.mult, ["Wim", "LRt"], ["fi"])
    tt("dve", t64c, t64a, LIt, ALU.mult, ["t64a", "LIt"], ["t64c"])
    tt("dve", fi, fi, t64c, ALU.subtract, ["fi", "t64c"], ["fi"])
    tt("dve", fi, fi, t64b, ALU.mult, ["fi", "t64b"], ["fi"])
    frb = fr[:, :, None].broadcast_to([128, 64, 8])
    fib = fi[:, :, None].broadcast_to([128, 64, 8])
    tt("dve", Gre, Wre[:, :, 7:15], frb, ALU.mult, ["Wre", "fr"], ["Gre"])
    tt("dve", Gt, Wim[:, :, 7:15], fib, ALU.mult, ["Wim", "fi"], ["Gt"])
    tt("dve", Gre, Gre, Gt, ALU.subtract, ["Gre", "Gt"], ["Gre"])
    tt("dve", Gim, Wre[:, :, 7:15], fib, ALU.mult, ["Wre", "fi"], ["Gim"])
    tt("dve", Gt, Wim[:, :, 7:15], frb, ALU.mult, ["Wim", "fr"], ["Gt"])
    tt("dve", Gim, Gim, Gt, ALU.add, ["Gim", "Gt"], ["Gim"])
    for r in range(2):
        hp = slice(r * 64, (r + 1) * 64)
        cp("dve", coefA[hp, :], Wre[hp, r * 32:(r + 1) * 32, 15], ["Wre"], ["coefA"])
        cp("dve", coefB[hp, 1, :], Wim[hp, r * 32:(r + 1) * 32, 15], ["Wim"], ["coefB"])
        ts("dve", coefB[hp, 0, :], Wim[hp, r * 32:(r + 1) * 32, 15], -1.0, None, ALU.mult, None, ["Wim"], ["coefB"])
    for dup in range(2):
        dma_in("pb%d" % dup, Bre_t[dup * 64:(dup + 1) * 64], b_re.rearrange("g p c -> p g c"), ["Bre_t"])
        dma_in("pb%d" % (2 + dup), Bim_t[dup * 64:(dup + 1) * 64], b_im.rearrange("g p c -> p g c"), ["Bim_t"])
    for ti in range(4):
        for ri, src in enumerate((c_re, c_im)):
            for dup in range(2):
                dma_in("pc%d%d" % (ri, dup), Cin[:, ti, ri, dup * 64:(dup + 1) * 64], src[ti * 128:(ti + 1) * 128, :], ["Cin"])
    for ti in range(4):
        tp(ps(1)[:, ti * 128:(ti + 1) * 128], Cin[:, ti, 0, :], ident_f, ["Cin", "ident_f"], ["ps1"])
        tp(ps(2)[:, ti * 128:(ti + 1) * 128], Cin[:, ti, 1, :], ident_f, ["Cin", "ident_f"], ["ps2"])
    cp("dve", Cre_t.rearrange("p g c -> p (g c)"), ps(1), ["ps1"], ["Cre_t"])
    cp("dve", Cim_t.rearrange("p g c -> p (g c)"), ps(2), ["ps2"], ["Cim_t"])
    for tau in range(8):
        dma_in("pd", Dcol[tau * 16:(tau + 1) * 16, :], s5_d.rearrange("(g c) -> c g", c=16), ["Dcol"], slow=True)
    V("pool", lambda e: e.memset(tmpA[:, 0, :, :], 1.0), [], ["tmpA"])
    V("pool", lambda e: e.affine_select(out=bmf, in_=tmpA[:, 0, :, :], pattern=[[16, 8], [0, 16]], compare_op=ALU.is_ge,
                                        fill=0.0, base=15, channel_multiplier=-1), ["tmpA"], ["bmf"])
    V("pool", lambda e: e.affine_select(out=bmb, in_=tmpA[:, 0, :, :], pattern=[[-16, 8], [0, 16]], compare_op=ALU.is_ge,
                                        fill=0.0, base=0, channel_multiplier=1), ["tmpA"], ["bmb"])

    def cplx_stack(dst, wre_v, wim_v, ca, cb, m_a, m_b, key, rows=slice(0, 128)):
        xa = XA[rows, 0:16, :]
        xb = XB[rows, 0:16, :]
        ta = tmpA[rows]
        tb = tmpB[rows]
        npart = rows.stop - rows.start

        def lin(o, m, okey):
            s0, s1 = m
            s0 = s0[rows] if not isinstance(s0, float) else s0
            s1 = s1[rows] if not isinstance(s1, float) else s1
            ts("dve", o, wre_v, s0, None, ALU.mult, None, ["Wre", "Wim", "Gre", "Gim", "RW", "m0", "m1", "nm0", "nm1"], [okey])
            stt("dve", o, wim_v, s1, o, ALU.mult, ALU.add, ["Wre", "Wim", "Gre", "Gim", "RW", "m0", "m1", "nm0", "nm1", okey], [okey])

        lin(xa, m_a, "XA")
        lin(xb, m_b, "XB")
        d = dst[rows]
        tt("dve", ta, xa[:, :, :, None].broadcast_to([npart, 16, 8, 16]),
           ca[:, :, None, :].broadcast_to([npart, 16, 8, 16]), ALU.mult,
           ["XA", "Bre_t", "Bim_t", "Cre_t", "Cim_t"], ["tmpA"])
        tt("pool", tb, xb[:, :, :, None].broadcast_to([npart, 16, 8, 16]),
           cb[:, :, None, :].broadcast_to([npart, 16, 8, 16]), ALU.mult,
           ["XB", "Bre_t", "Bim_t", "Cre_t", "Cim_t"], ["tmpB"])
        tt("dve", d, ta, tb, ALU.add, ["tmpA", "tmpB"], [key])

    for which, (src_w, dst_d, ncols) in enumerate(((w_in, winb_d, D_IN), (w_out, woutb_d, D))):
        for c in range(8):
            sl = c % 2
            sap = stg[:, sl, 0:ncols]
            dma_in("stg%d" % sl, sap, src_w[c * 128:(c + 1) * 128, :], ["stg%d" % sl])
            cb_ = cvb[:, sl, 0:ncols]
            if which == 0:
                act(cb_, sap, AF.Copy, ["stg%d" % sl, "gpre_c"], ["cvb%d" % sl], scale=gpre_c[:, c:c + 1])
            else:
                cp("act", cb_, sap, ["stg%d" % sl], ["cvb%d" % sl])
            P.dma("sp", "cvw%d" % sl, lambda e, dst_d=dst_d, c=c, cb_=cb_: e.dma_start(out=dst_d[c], in_=cb_),
                  reads=["cvb%d" % sl], writes=["wscr%d" % which])

    bmf2 = bmf.rearrange("p a b -> p (a b)")
    bmb2 = bmb.rearrange("p a b -> p (a b)")
    A_ = slice(0, 128)
    for gh in range(2):
        g0 = gh * 16
        gg_ = slice(g0, g0 + 16)
        for r in range(2):
            gs = slice(r * 32 + g0, r * 32 + g0 + 16)
            if r == 0:
                pw_re, pw_im = Gre[:, gs, ::-1], Gim[:, gs, ::-1]
                hw_re, hw_im = Wre[:, gs, 0:8], Wim[:, gs, 0:8]
                qw_re, qw_im = Wre[:, gs, 8:16], Wim[:, gs, 8:16]
            else:
                pw_re, pw_im = Gre[:, gs, :], Gim[:, gs, :]
                hw_re, hw_im = Wre[:, gs, 7::-1], Wim[:, gs, 7::-1]
                qw_re, qw_im = Wre[:, gs, 15:7:-1], Wim[:, gs, 15:7:-1]
            cplx_stack(Pst[:, r], pw_re, pw_im, Bre_t[:, gg_], Bim_t[:, gg_], (m0, m1), (m1, nm0), "Pst")
            cplx_stack(Hst[:, r], hw_re, hw_im, Cre_t[:, gg_], Cim_t[:, gg_], (m0, nm1), (nm1, nm0), "Hst")
            hp = slice(r * 64, (r + 1) * 64)
            for ti, tv in enumerate((qw_re, qw_im, pw_re, pw_im)):
                cp("dve", RW[hp, ti], tv[hp], ["Wre", "Wim", "Gre", "Gim"], ["RW"])
        cplx_stack(Qre_s, RW[:, 0], RW[:, 1], Cre_t[:, gg_], Cim_t[:, gg_], (1.0, 0.0), (0.0, -1.0), "Qre_s")
        cplx_stack(Qim_s, RW[:, 0], RW[:, 1], Cre_t[:, gg_], Cim_t[:, gg_], (0.0, -1.0), (-1.0, 0.0), "Qim_s")
        cplx_stack(Pre_s, RW[:, 2], RW[:, 3], Bre_t[:, gg_], Bim_t[:, gg_], (1.0, 0.0), (0.0, -1.0), "Pre_s")
        cplx_stack(Pim_s, RW[:, 2], RW[:, 3], Bre_t[:, gg_], Bim_t[:, gg_], (0.0, 1.0), (1.0, 0.0), "Pim_s")

        for gl in range(16):
            g = g0 + gl
            sl = g % 2
            o5 = s5o[:, sl]
            okey = "s5o%d" % sl
            pf = Pst[:, 0, gl].rearrange("p a b -> p (a b)")
            hf = Hst[:, 0, gl].rearrange("p a b -> p (a b)")
            pb_ = Pst[:, 1, gl].rearrange("p a b -> p (a b)")
            hb_ = Hst[:, 1, gl].rearrange("p a b -> p (a b)")
            mm(ps(3)[:, 0:128], pf, hf, True, True, ["Pst", "Hst"], ["ps3"])
            mm(ps(3)[:, 128:256], pb_, hb_, True, True, ["Pst", "Hst"], ["ps3"])
            tt("dve", tT1, ps(3)[:, 0:128], bmf2, ALU.mult, ["ps3", "bmf"], ["tT1"])
            tt("dve", tT2, ps(3)[:, 128:256], bmb2, ALU.mult, ["ps3", "bmb"], ["tT2"])
            tt("dve", tT1, tT1, tT2, ALU.add, ["tT1", "tT2"], ["tT1"])
            stt("dve", o5[:, 0, :], ident_f, Dcol[:, g:g + 1], tT1, ALU.mult, ALU.add, ["ident_f", "Dcol", "tT1"], [okey])
            srcs = (Pre_s, Pim_s)
            for k in range(2):
                tp(ps(4)[:, k * 128:(k + 1) * 128], srcs[k][:, gl].rearrange("p a b -> p (a b)"), ident_f,
                   ["Pre_s", "Pim_s", "ident_f"], ["ps4"])
            cp("act", o5[:, 1:3, :], ps(4)[:, 0:256].rearrange("p (a b) -> p a b", b=128), ["ps4"], [okey])
            cp("dve", o5[:, 3, :], Qre_s[:, gl].rearrange("p a b -> p (a b)"), ["Qre_s"], [okey])
            cp("dve", o5[:, 4, :], Qim_s[:, gl].rearrange("p a b -> p (a b)"), ["Qim_s"], [okey])
            P.dma("sp", "s5w%d" % sl, lambda e, g=g, o5=o5: e.dma_start(out=s5m_d[g], in_=o5.rearrange("p a b -> p (a b)")),
                  reads=[okey], writes=["s5m_d"])

    P.barrier()
    AR.off = mark

    wslot_0 = AR.off
    wslot = AR.alloc([8, D_IN], BF)
    wslot_1 = AR.off
    stgA = stg
    xt = AR.alloc([2, D], F32)
    xnb2_ = AR.alloc([2, D], BF)
    ssq = AR.alloc([4], F32)
    ssqI = AR.alloc([2], F32)
    regII_0 = AR.off
    big32 = AR.alloc([16384], BF)
    hT = big32[:, 0:8 * S].rearrange("p (c t) -> p c t", t=S)
    VS = big32[:, 0:64 * NCH].rearrange("p (r g k) -> p r g k", g=32, k=NCH)
    XX = AR.alloc([NHB, 8, 512], BF)
    XXg = XX.rearrange("p h t f -> p h (t f)").rearrange("p h (g t c) -> p h g t c", g=32, t=8)
    Ucol = AR.alloc([32, NCH], BF)
    regII_1 = AR.off
    xmT = AR.alloc([4, S + 4], BF)
    sigoT = AR.alloc([4, S], BF)
    yT = AR.alloc([8, S], BF)
    gates = AR.alloc([NT, 16], F32)
    lfn = AR.alloc([NT, 8], F32)
    e1 = AR.alloc([NT, 8], F32)
    e2 = AR.alloc([NT, 8], F32)
    e3 = AR.alloc([NT, 8], F32)
    eBL = AR.alloc([NT, 8], F32)
    _save = AR.off
    AR.off = wslot_0
    s5m = AR.alloc([2, 5, 128], BF)
    Z = AR.alloc([2, 3, 32], F32)
    zt1 = AR.alloc([2, 32], F32)
    zt2 = AR.alloc([2, 32], F32)
    gl_sq2 = AR.alloc([2, NCH], F32)
    gl_in2 = AR.alloc([2, NCH], F32)
    ygc2 = AR.alloc([2, NCH], BF)
    sgt2 = AR.alloc([2, 4, 512], BF)
    VSn = AR.alloc([2, 2, NCH], BF)
    assert AR.off <= wslot_1
    AR.off = wslot_0
    Sm = AR.alloc([2, 128], BF)
    Cf = AR.alloc([2, 129], F32)
    Cb = AR.alloc([2, 2, 129], BF)
    dnv = AR.alloc([2, 3, NT], F32)
    lnv2 = AR.alloc([2, NT], F32)
    lnv3 = AR.alloc([2, NT], F32)
    hnb_all = AR.alloc([NT, 128], BF)
    otmp = AR.alloc([1024], F32)
    assert AR.off <= wslot_1
    AR.off = regII_0
    cacc = AR.alloc([S], F32)
    xc = AR.alloc([S], BF)
    xcs = AR.alloc([S], BF)
    qT = AR.alloc([S], BF)
    kT = AR.alloc([S], BF)
    ktok = AR.alloc([NT, 128], BF)
    vtall = AR.alloc([NT, 4, 129], BF)
    esc = AR.alloc([NT, 4], F32)
    nd = AR.alloc([2, NT, 129], F32)
    hacc = cacc.rearrange("p (i d) -> p i d", d=128)
    assert AR.off <= regII_1, (AR.off, regII_1)
    AR.off = regII_0
    x1t2 = AR.alloc([2, D], F32)
    ytmp2 = AR.alloc([2, D], F32)
    ssqO = AR.alloc([2, 4], F32)
    AR.off = _save
    phaseA_end = AR.off

    V("pool", lambda e: e.memset(xmT, 0.0), [], ["xmT"])
    VSK = [("VS", k) for k in range(NCH)]

    def rstd_from_ss(ss_ap, n, key):
        act(ss_ap, ss_ap, AF.Sqrt, [key, "eps_c"], [key], scale=1.0 / n, bias=eps_c)
        V("dve", lambda e: e.reciprocal(out=ss_ap, in_=ss_ap), [key], [key])

    def norm_transpose_tile(src_ap, src_key, dstT, dst_key, col0, bank):
        par = bank % 2
        xnb = xnb2_[:, par, :]
        kx, ks = "xnb%d" % par, "ssqI%d" % par
        sq = ssqI[:, par:par + 1]
        act(xnb, src_ap, AF.Square, [src_key], [kx, ks], accum=sq)
        rstd_from_ss(sq, D, ks)
        act(xnb, src_ap, AF.Copy, [src_key, ks], [kx], scale=sq)
        pb = psbf(bank)
        for c in range(8):
            tp(pb[:, c * 128:(c + 1) * 128], xnb[:, c * 128:(c + 1) * 128], ident_b, [kx, "ident_b"], ["ps%d" % bank])
        cp("dve", dstT[:, :, col0:col0 + 128], pb.rearrange("p (c t) -> p c t", t=128), ["ps%d" % bank], [dst_key])

    for b in range(nseq):
        t0 = b * S
        dma_in("winl", wslot, winb_d.rearrange("c p n -> p c n"), ["wslot"], reads=["wscr0"])
        cnt = 0
        for tb in range(NTB):
            hk = ("hT", tb)
            for i in range(tb * 4, tb * 4 + 4):
                sl = i % 2
                dma_in("x%d" % sl, xt[:, sl, :], x[t0 + i * 128:t0 + (i + 1) * 128, :], ["xt%d" % sl])
                norm_transpose_tile(xt[:, sl, :], "xt%d" % sl, hT, hk, i * 128, 2 + (i % 2))
            for h in range(4):
                for which in range(2):
                    bank = cnt % 2
                    cnt += 1
                    col0 = 512 + which * 512 + h * 128
                    for c in range(8):
                        mm(ps(bank), wslot[:, c, col0:col0 + 128], hT[:, c, tb * 512:(tb + 1) * 512], c == 0, c == 7,
                           [hk, "wslot"], ["ps%d" % bank])
                    if which == 0:
                        cp("dve", xmT[:, h, 2 + tb * 512:2 + (tb + 1) * 512], ps(bank), ["ps%d" % bank], ["xmT"])
                    else:
                        act(sigoT[:, h, tb * 512:(tb + 1) * 512], ps(bank), AF.Sigmoid, ["ps%d" % bank], ["sigoT"])
            for i in range(tb * 4, tb * 4 + 4):
                for c in range(8):
                    mm(ps(4)[:, i * 16:(i + 1) * 16], hT[:, c, i * 128:(i + 1) * 128], wslot[:, c, 1536:1552], c == 0, c == 7,
                       [hk, "wslot"], ["ps4"])
            if tb % 2 == 1:
                hb = tb // 2
                hks = [("hT", tb - 1), ("hT", tb)]
                for tau in range(8):
                    bank = tau % 2
                    for c in range(8):
                        mm(ps(bank), hT[:, c, hb * 1024 + tau:(hb + 1) * 1024:8], wslot[:, c, 0:512], c == 0, c == 7,
                           hks + ["wslot"], ["ps%d" % bank])
                    xdst = XXg[:, hb, :, tau, :]
                    psrc = ps(bank).rearrange("p (g c) -> p g c", c=16)
                    if tau % 2 == 0:
                        cp("dve", xdst, psrc, ["ps%d" % bank], ["XX%d" % hb])
                    else:
                        cp("act", xdst, psrc, ["ps%d" % bank], ["XX%d" % hb])
                for gq in range(4):
                    bank = 2 + gq % 2
                    pb = psbf(bank)
                    for gg in range(8):
                        g = gq * 8 + gg
                        tp(pb[:, gg * 128:(gg + 1) * 128], XXg[:, hb, g].rearrange("p a b -> p (a b)"), ident_b,
                           ["XX%d" % hb, "ident_b"], ["ps%d" % bank])
                    cp("dve", Ucol[:, gq * 8:(gq + 1) * 8, hb * 128:(hb + 1) * 128],
                       pb.rearrange("p (g k) -> p g k", k=128), ["ps%d" % bank], ["Ucol"])
        tt("dve", gates, ps(4)[:, 0:NT * 16].rearrange("p (i c) -> p i c", c=16),
           gbias[:, None, :].broadcast_to([128, NT, 16]), ALU.add, ["ps4", "gbias"], ["gates"])
        act(lfn, gates[:, :, 8:16], AF.Exp, ["gates"], ["lfn"], scale=-1.0)
        act(lfn, lfn, AF.Ln, ["lfn", "one_c"], ["lfn"], bias=one_c)
        ts("dve", lfn, lfn, -1.0, None, ALU.mult, None, ["lfn"], ["lfn"])
        for i in range(NT):
            mm(ps(5)[:, i * 16:i * 16 + 4], tri_f, lfn[:, i, 0:4], True, True, ["tri_f", "lfn"], ["ps5"])
            mm(ps(5)[:, i * 16 + 4:i * 16 + 8], tri_b, lfn[:, i, 4:8], True, True, ["tri_b", "lfn"], ["ps5"])
            mm(ps(5)[:, i * 16 + 8:i * 16 + 16], ones_f, lfn[:, i, 0:8], True, True, ["ones_f", "lfn"], ["ps5"])
        p5 = ps(5)[:, 0:NT * 16].rearrange("p (i c) -> p i c", c=16)
        act(e1, p5[:, :, 0:8], AF.Exp, ["ps5"], ["e1"])
        tt("dve", e2, gates[:, :, 0:8], p5[:, :, 0:8], ALU.subtract, ["gates", "ps5"], ["e2"])
        act(e2, e2, AF.Exp, ["e2"], ["e2"])
        act(eBL, p5[:, :, 8:16], AF.Exp, ["ps5"], ["eBL"])
        tt("dve", e3, e2, eBL, ALU.mult, ["e2", "eBL"], ["e3"])

        P.barrier()
        for g in range(32):
            sl = g % 2
            dma_in("s5l%d" % sl, s5m[:, sl].rearrange("p a b -> p (a b)"), s5m_d[g], ["s5m%d" % sl], reads=["s5m_d"])
            bank = g % 2
            mm(ps(bank)[:, 0:NCH], s5m[:, sl, 1, :], Ucol[:, g, :], True, True, ["s5m%d" % sl, "Ucol"], ["ps%d" % bank])
            mm(ps(bank)[:, 256:256 + NCH], s5m[:, sl, 2, :], Ucol[:, g, :], True, True, ["s5m%d" % sl, "Ucol"], ["ps%d" % bank])
            src = ps(bank).rearrange("p (r k) -> p r k", k=256)[:, :, 0:NCH]
            cp("act", VS[0:64, :, g, :], src[0:64], ["ps%d" % bank], ["big32"] + VSK)
            cp("dve", VS[64:128, :, g, :], src[64:128, :, ::-1], ["ps%d" % bank], ["big32"] + VSK)
        Zb = Z.rearrange("p a b c -> p (a b c)").rearrange("p (n s g) -> p n s g", n=3, s=2)
        V("dve", lambda e: e.memset(Zb[:, 0], 0.0), [], ["Z0"])
        Ab = coefA[:, None, :].broadcast_to([128, 2, 32])
        for step in range(NCH):
            zi, zo = step % 3, (step + 1) % 3
            zin, zout = Zb[:, zi], Zb[:, zo]
            tt("dve", zt1, Ab, zin, ALU.mult, ["coefA", "Z%d" % zi], ["zt1"])
            tt("dve", zt2, coefB, zin[:, 1::-1, :], ALU.mult, ["coefB", "Z%d" % zi], ["zt2"])
            tt("dve", zt1, zt1, zt2, ALU.add, ["zt1", "zt2"], ["zt1"])
            tt("dve", zout, zt1, VS[:, :, :, step], ALU.add, ["zt1", ("VS", step)], ["Z%d" % zo])
            cp("act", VS[:, :, :, step], zin, ["Z%d" % zi], [("VS", step)])
        def y_matmuls(g):
            sl = g % 2
            dma_in("s5l%d" % sl, s5m[:, sl].rearrange("p a b -> p (a b)"), s5m_d[g], ["s5m%d" % sl], reads=["s5m_d"])
            bank = g % 2
            py = ps(bank)[:, 0:NCH]
            mm(py, s5m[:, sl, 0, :], Ucol[:, g, :], True, False, ["s5m%d" % sl, "Ucol"], ["ps%d" % bank])
            cp("act", VSn[0:64, sl], VS[0:64, :, g, :], ["big32"] + VSK, ["VSn%d" % sl])
            cp("dve", VSn[64:128, sl], VS[64:128, :, g, ::-1], ["big32"] + VSK, ["VSn%d" % sl])
            mm(py, s5m[:, sl, 3, :], VSn[:, sl, 0, :], False, False, ["s5m%d" % sl, "VSn%d" % sl], ["ps%d" % bank])
            mm(py, s5m[:, sl, 4, :], VSn[:, sl, 1, :], False, True, ["s5m%d" % sl, "VSn%d" % sl], ["ps%d" % bank])

        y_matmuls(0)
        for g in range(32):
            sl = g % 2
            bank = g % 2
            py = ps(bank)[:, 0:NCH]
            if g + 1 < 32:
                y_matmuls(g + 1)
            ygc = ygc2[:, sl, :]
            kyg = "ygc%d" % sl
            act(ygc, py, AF.Gelu_apprx_tanh, ["ps%d" % bank], [kyg])
            pb = psbf(2 + g % 2)
            for hb in range(NHB):
                tp(pb[:, hb * 128:(hb + 1) * 128], ygc[:, hb * 128:(hb + 1) * 128], ident_b, [kyg, "ident_b"], ["ps%d" % (2 + g % 2)])
            for hb in range(NHB):
                cp("dve", XX[:, hb, :, g * 16:(g + 1) * 16],
                   pb[:, hb * 128:(hb + 1) * 128].rearrange("p (a b) -> p a b", b=16), ["ps%d" % (2 + g % 2)], ["XX%d" % hb])
        cnt = 0
        for hb in range(NHB):
            for ft in range(4):
                bank = 2 + cnt % 2
                cnt += 1
                pb = psbf(bank)
                for tau in range(8):
                    tp(pb[:, tau * 128:(tau + 1) * 128], XX[:, hb, tau, ft * 128:(ft + 1) * 128], ident_b,
                       ["XX%d" % hb, "ident_b"], ["ps%d" % bank])
                cp("dve" if cnt % 2 else "act", yT[:, ft, hb * 1024:(hb + 1) * 1024].rearrange("p (k t) -> p t k", t=8),
                   pb.rearrange("p (t k) -> p t k", k=128), ["ps%d" % bank], ["yT"])
        for tb in range(NTB):
            sgt = sgt2[:, tb % 2]
            ksg = "sgt%d" % (tb % 2)
            for fo in range(4):
                bank = fo % 2
                for ft in range(4):
                    mm(ps(bank), wglub[:, ft, fo * 128:(fo + 1) * 128], yT[:, ft, tb * 512:(tb + 1) * 512], ft == 0, ft == 3,
                       ["wglub", "yT", ("yTg", tb)], ["ps%d" % bank])
                act(sgt[:, fo, :], ps(bank), AF.Sigmoid, ["ps%d" % bank, "bglu_c"], [ksg], bias=bglu_c[:, fo:fo + 1])
            tt("dve", yT[:, 0:4, tb * 512:(tb + 1) * 512], yT[:, 0:4, tb * 512:(tb + 1) * 512], sgt, ALU.mult,
               ["yT", ksg], [("yTg", tb)])

        P.barrier()
        for h in range(4):
            for tb in range(NTB):
                bank = tb % 2
                for k in range(5):
                    mm(ps(bank), dgc[:, h, k, :], xmT[:, h, tb * 512 + k:tb * 512 + k + 512], k == 0, k == 4,
                       ["dgc", "xmT"], ["ps%d" % bank])
                act(xc[:, tb * 512:(tb + 1) * 512], ps(bank), AF.Silu, ["ps%d" % bank, "convb_c"], ["xc"], bias=convb_c[:, h:h + 1])
            ts("dve", xcs, xc, skip_c[:, h:h + 1], None, ALU.mult, None, ["xc", "skip_c"], ["xcs"])
            for tb in range(NTB):
                cs = slice(tb * 512, (tb + 1) * 512)
                mm(ps(0), wqb[:, h, :], xc[:, cs], True, True, ["wqb", "xc"], ["ps0"])
                cp("act", qT[:, cs], ps(0), ["ps0"], ["qT"])
                mm(ps(1), wkb[:, h, :], xc[:, cs], True, True, ["wkb", "xc"], ["ps1"])
                cp("dve", kT[:, cs], ps(1), ["ps1"], ["kT"])
            for r in range(2):
                cp("dve", esc[:, :, r], e2[:, :, r * 4 + h], ["e2"], ["esc"])
                cp("dve", esc[:, :, 2 + r], e3[:, :, r * 4 + h], ["e3"], ["esc"])
            for i in range(NT):
                bank = i % 2
                ts_ = slice(i * 128, (i + 1) * 128)
                mm(ps(bank)[:, 0:128], xc[:, ts_], wkb[:, h, :], True, True, ["wkb", "xc"], ["ps%d" % bank])
                mm(ps(bank)[:, 128:256], xmT[:, h, 2 + i * 128:2 + (i + 1) * 128], wvb[:, h, :], True, True, ["wvb", "xmT"], ["ps%d" % bank])
                cp("act", ktok[:, i, :], ps(bank)[:, 0:128], ["ps%d" % bank], ["ktok"])
                tt("dve", vtall[:, i, :, 0:128], ps(bank)[:, 128:256][:, None, :].broadcast_to([128, 4, 128]),
                   esc[:, i, :][:, :, None].broadcast_to([128, 4, 128]), ALU.mult, ["ps%d" % bank, "esc"], ["vtall"])
            cp("dve", vtall[:, :, :, 128], esc, ["esc"], ["vtall"])
            def rec_info(cc):
                info = []
                for r in range(2):
                    c = cc if r == 0 else NT - 1 - cc
                    bS = (6, 7)[cc % 2] if r == 0 else (2, 3)[cc % 2]
                    info.append((r, c, r * 4 + h, slice(c * 128, (c + 1) * 128), bS))
                return info

            def rec_front(cc):
                for (r, c, rh, cs, bS) in rec_info(cc):
                    mm(ps(bS)[:, 0:128], kT[:, cs], qT[:, cs], True, True, ["kT", "qT"], ["ps%d" % bS])
                    if cc < NT - 1:
                        mm(ps(bS)[:, 260:389], ktok[:, c, :], vtall[:, c, 2 + r, :], True, True, ["ktok", "vtall"], ["ps%d" % bS])

            rec_front(0)
            for cc in range(NT):
                info = rec_info(cc)
                if cc + 1 < NT:
                    rec_front(cc + 1)
                for (r, c, rh, cs, bS) in info:
                    if cc < NT - 1:
                        pc = ps(bS)[:, 260:389]
                        if cc == 0:
                            cp("dve", Cf[:, r, :], pc, ["ps%d" % bS], ["Cf%d" % r])
                        else:
                            stt("dve", Cf[:, r, :], Cf[:, r, :], eBL[:, c, rh:rh + 1], pc, ALU.mult, ALU.add,
                                ["Cf%d" % r, "eBL", "ps%d" % bS], ["Cf%d" % r])
                        cp("act", Cb[:, r, (cc + 1) % 2, :], Cf[:, r, :], ["Cf%d" % r], ["Cb%d%d" % (r, (cc + 1) % 2)])
                    msk = msk_f if r == 0 else msk_b
                    tt("dve", Sm[:, r, :], ps(bS)[:, 0:128], msk, ALU.mult, ["ps%d" % bS, "msk_f", "msk_b"], ["Sm%d" % r])
                for (r, c, rh, cs, bS) in info:
                    pn = ps(bS)[:, 128:257]
                    mm(pn, Sm[:, r, :], vtall[:, c, r, :], True, cc == 0, ["Sm%d" % r, "vtall"], ["ps%d" % bS])
                    if cc > 0:
                        mm(pn, qT[:, cs], Cb[:, r, cc % 2, :], False, True, ["qT", "Cb%d%d" % (r, cc % 2)], ["ps%d" % bS])
                    cp("act", nd[:, r, c, :], pn, ["ps%d" % bS], ["nd%d" % r])
            for r in range(2):
                rh = r * 4 + h
                den = nd[:, r, :, 128]
                e1r = e1[:, :, rh]
                dA, dB, dC = dnv[:, r, 0, :], dnv[:, r, 1, :], dnv[:, r, 2, :]
                tt("dve", dA, den, e1r, ALU.mult, ["nd%d" % r, "e1"], ["dnv%d" % r])
                ts("dve", dB, dA, -1.0, None, ALU.mult, None, ["dnv%d" % r], ["dnv%d" % r])
                tt("dve", dA, dA, dB, ALU.max, ["dnv%d" % r], ["dnv%d" % r])
                ts("dve", dA, dA, 1.0, None, ALU.max, None, ["dnv%d" % r], ["dnv%d" % r])
                V("dve", lambda e, dA=dA: e.reciprocal(out=dA, in_=dA), ["dnv%d" % r], ["dnv%d" % r])
                tt("dve", dC, dA, e1r, ALU.mult, ["dnv%d" % r, "e1"], ["dnv%d" % r])
            sc0 = dnv[:, 0, 2, :][:, :, None].broadcast_to([128, NT, 128])
            sc1 = dnv[:, 1, 2, :][:, :, None].broadcast_to([128, NT, 128])
            tt("dve", nd[:, 1, :, 0:128], nd[:, 1, :, 0:128], sc1, ALU.mult, ["nd1", "dnv1"], ["nd1"])
            tt("dve", hacc, nd[:, 0, :, 0:128], sc0, ALU.mult, ["nd0", "dnv0"], ["cacc"])
            tt("dve", hacc, hacc, nd[:, 1, :, 0:128], ALU.add, ["cacc", "nd1"], ["cacc"])
            V("dve", lambda e: e.tensor_reduce(out=lnv2[:, 0, :], in_=hacc, axis=mybir.AxisListType.X, op=ALU.add),
              ["cacc"], ["lnv2a"])
            for i in range(NT):
                act(hnb_all[:, i, :], hacc[:, i, :], AF.Square, ["cacc"], ["hnb_all", "lnv2b"], accum=lnv2[:, 1, i:i + 1])
            ts("dve", lnv2[:, 0, :], lnv2[:, 0, :], -1.0 / 128, None, ALU.mult, None, ["lnv2a"], ["lnv2a"])
            tt("dve", lnv3[:, 0, :], lnv2[:, 0, :], lnv2[:, 0, :], ALU.mult, ["lnv2a"], ["lnv3a"])
            stt("dve", lnv2[:, 1, :], lnv2[:, 1, :], 1.0 / 128, lnv3[:, 0, :], ALU.mult, ALU.subtract, ["lnv2b", "lnv3a"], ["lnv2b"])
            ts("dve", lnv2[:, 1, :], lnv2[:, 1, :], EPS, None, ALU.add, None, ["lnv2b"], ["lnv2b"])
            act(lnv2[:, 1, :], lnv2[:, 1, :], AF.Sqrt, ["lnv2b"], ["lnv2b"])
            V("dve", lambda e: e.reciprocal(out=lnv2[:, 1, :], in_=lnv2[:, 1, :]), ["lnv2b"], ["lnv2b"])
            tt("dve", lnv3[:, 1, :], lnv2[:, 0, :], lnv2[:, 1, :], ALU.mult, ["lnv2a", "lnv2b"], ["lnv3b"])
            for i in range(NT):
                act(hnb_all[:, i, :], hacc[:, i, :], AF.Identity, ["cacc", "lnv2b", "lnv3b"], ["hnb_all"],
                    scale=lnv2[:, 1, i:i + 1], bias=lnv3[:, 1, i:i + 1])
            GT = min(NT, 8)
            for jb in range(NT // GT):
                bank = 4 + jb % 2
                pb = psbf(bank)
                for ii in range(GT):
                    i = jb * GT + ii
                    tp(pb[:, ii * 128:(ii + 1) * 128], hnb_all[:, i, :], ident_b, ["hnb_all", "ident_b"], ["ps%d" % bank])
                cols = slice(jb * GT * 128, (jb + 1) * GT * 128)
                w = GT * 128
                stt("dve", otmp[:, 0:w], pb[:, 0:w], hnorm_c[:, h:h + 1], xcs[:, cols], ALU.mult, ALU.add,
                    ["ps%d" % bank, "hnorm_c", "xcs"], ["otmp"])
                tt("pool", yT[:, 4 + h, cols], otmp[:, 0:w], sigoT[:, h, cols], ALU.mult, ["otmp", "sigoT"], ["yT"])

        P.barrier()
        dma_in("woutl", wslot[:, :, 0:D], woutb_d.rearrange("c p n -> p c n"), ["wslot"], reads=["wscr1"])
        for i in range(NT):
            ts_ = slice(i * 128, (i + 1) * 128)
            sl = i % 2
            x1t, ytmp, sq3 = x1t2[:, sl, :], ytmp2[:, sl, :], ssqO[:, sl, :]
            kx1, kyt, ksq = "x1t%d" % sl, "ytmp%d" % sl, "ssqO%d" % sl
            dma_in("x%d" % sl, xt[:, sl, :], x[t0 + i * 128:t0 + (i + 1) * 128, :], ["xt%d" % sl])
            bks = (0, 1) if sl == 0 else (2, 3)
            for half in range(2):
                bk = bks[half]
                for ft in range(8):
                    mm(ps(bk), yT[:, ft, ts_], wslot[:, ft, half * 512:(half + 1) * 512], ft == 0, ft == 7,
                       ["yT", "wslot"] + [("yTg", t_) for t_ in range(NTB)], ["ps%d" % bk])
                act(ytmp[:, half * 512:(half + 1) * 512], ps(bk), AF.Square, ["ps%d" % bk], [kyt, ksq], accum=sq3[:, 1 + half:2 + half])
            tt("dve", sq3[:, 1:2], sq3[:, 1:2], sq3[:, 2:3], ALU.add, [ksq], [ksq])
            rstd_from_ss(sq3[:, 1:2], D, ksq)
            for half in range(2):
                hs = slice(half * 512, (half + 1) * 512)
                stt("dve", ytmp[:, hs], ps(bks[half]), sq3[:, 1:2], gpost[:, hs], ALU.mult, ALU.mult, ["ps%d" % bks[half], ksq, "gpost"], [kyt])
            tt("pool", x1t[:, 0:512], ytmp[:, 0:512], xt[:, sl, 0:512], ALU.add, [kyt, "xt%d" % sl], [kx1])
            tt("dve", x1t[:, 512:1024], ytmp[:, 512:1024], xt[:, sl, 512:1024], ALU.add, [kyt, "xt%d" % sl], [kx1 + "b"])
            P.dma("sp", "x1w%d" % sl, lambda e, i=i, t0=t0, x1t=x1t: e.dma_start(out=x1_d[t0 + i * 128:t0 + (i + 1) * 128, :], in_=x1t),
                  reads=[kx1, kx1 + "b"], writes=["x1_d"])
        P.barrier()

    P.barrier()
    AR.off = persist_end
    stgB = AR.alloc([2, 1408], F32)
    wgb = AR.alloc([8, D_FF], BF)
    wub = AR.alloc([8, D_FF], BF)
    wdb = AR.alloc([NFF, D], BF)
    x1b = AR.alloc([4, D], F32)
    xnb2 = AR.alloc([D], BF)
    ssq2 = AR.alloc([4], F32)
    ssqF2 = AR.alloc([4], F32)
    h2T = AR.alloc([8, 512], BF)
    sgf = AR.alloc([2, 512], BF)
    actT = AR.alloc([NFF, 512], BF)
    otile_a = AR.alloc([D], F32)
    otile_b = stgB[:, 0, 0:D]
    otiles = (otile_a, otile_b)

    def load_convert_b(src_rows, ncols, dst, scale_col, keyw):
        for i, src in enumerate(src_rows):
            sl = i % 2
            sap = stgB[:, sl, 0:ncols]
            dma_in("stgB%d" % sl, sap, src, ["stgB%d" % sl])
            d_ap = dst(i)
            if scale_col is not None:
                sc = scale_col(i)
                if i % 2 == 0:
                    V("act", lambda e, d_ap=d_ap, sap=sap, sc=sc: e.activation(out=d_ap, in_=sap, func=AF.Copy, scale=sc),
                      ["stgB%d" % sl, "gfpre_c"], [keyw])
                else:
                    V("dve", lambda e, d_ap=d_ap, sap=sap, sc=sc: e.tensor_scalar(out=d_ap, in0=sap, scalar1=sc, scalar2=None, op0=ALU.mult),
                      ["stgB%d" % sl, "gfpre_c"], [keyw])
            else:
                if i % 2 == 0:
                    V("act", lambda e, d_ap=d_ap, sap=sap: e.activation(out=d_ap, in_=sap, func=AF.Copy), ["stgB%d" % sl], [keyw])
                else:
                    V("dve", lambda e, d_ap=d_ap, sap=sap: e.tensor_copy(out=d_ap, in_=sap), ["stgB%d" % sl], [keyw])

    HF = D_FF // 2
    load_convert_b([w_gate[(i // 2) * 128:(i // 2 + 1) * 128, (i % 2) * HF:(i % 2 + 1) * HF] for i in range(16)], HF,
                   lambda i: wgb[:, i // 2, (i % 2) * HF:(i % 2 + 1) * HF], lambda i: gfpre_c[:, i // 2:i // 2 + 1], "wgb")
    load_convert_b([w_up[(i // 2) * 128:(i // 2 + 1) * 128, (i % 2) * HF:(i % 2 + 1) * HF] for i in range(16)], HF,
                   lambda i: wub[:, i // 2, (i % 2) * HF:(i % 2 + 1) * HF], lambda i: gfpre_c[:, i // 2:i // 2 + 1], "wub")
    load_convert_b([w_down[c * 128:(c + 1) * 128, :] for c in range(NFF)], D, lambda i: wdb[:, i, :], None, "wdb")

    def norm_transpose_b(src_ap, src_key, col0, bank):
        act(xnb2, src_ap, AF.Square, [src_key], ["xnb2", "ssq2"], accum=ssq2[:, 0:1])
        act(ssq2[:, 0:1], ssq2[:, 0:1], AF.Sqrt, ["ssq2", "eps_c"], ["ssq2"], scale=1.0 / D, bias=eps_c)
        V("dve", lambda e: e.reciprocal(out=ssq2[:, 0:1], in_=ssq2[:, 0:1]), ["ssq2"], ["ssq2"])
        act(xnb2, src_ap, AF.Copy, [src_key, "ssq2"], ["xnb2"], scale=ssq2[:, 0:1])
        pb = psbf(bank)
        for c in range(8):
            tp(pb[:, c * 128:(c + 1) * 128], xnb2[:, c * 128:(c + 1) * 128], ident_b, ["xnb2", "ident_b"], ["ps%d" % bank])
        cp("dve", h2T[:, :, col0:col0 + 128], pb.rearrange("p (c t) -> p c t", t=128), ["ps%d" % bank], ["h2T"])

    P.barrier()
    ocnt = 0
    xin = stgB[:, 1, 0:D]
    NBLK = NTOK // 512

    def prep_block(blk):
        for j in range(4):
            r0 = blk * 512 + j * 128
            dma_in("xinl", xin, x1_d[r0:r0 + 128, :], ["xin"], reads=["x1_d"])
            norm_transpose_b(xin, "xin", j * 128, j % 2)

    prep_block(0)
    for blk in range(NBLK):
        for j in range(4):
            r0 = blk * 512 + j * 128
            dma_in("x1l%d" % j, x1b[:, j, :], x1_d[r0:r0 + 128, :], ["x1b%d" % j], reads=["x1_d"])
        for f in range(NFF):
            bg, bu = (0, 1) if f % 2 == 0 else (2, 3)
            for c in range(8):
                mm(ps(bg), wgb[:, c, f * 128:(f + 1) * 128], h2T[:, c, :], c == 0, c == 7, ["wgb", "h2T"], ["ps%d" % bg])
            for c in range(8):
                mm(ps(bu), wub[:, c, f * 128:(f + 1) * 128], h2T[:, c, :], c == 0, c == 7, ["wub", "h2T"], ["ps%d" % bu])
            act(sgf[:, f % 2, :], ps(bg), AF.Silu, ["ps%d" % bg], ["sgf%d" % (f % 2)])
            tt("dve", actT[:, f, :], sgf[:, f % 2, :], ps(bu), ALU.mult, ["sgf%d" % (f % 2), "ps%d" % bu], ["actT"])
        if blk + 1 < NBLK:
            prep_block(blk + 1)
        for j in range(4):
            r0 = blk * 512 + j * 128
            osl = ocnt % 2
            ocnt += 1
            otile = otiles[osl]
            bks = (4, 5) if osl == 0 else (6, 7)
            ksq = "ssqF%d" % osl
            sqF = ssq2[:, 0:4] if osl == 0 else ssqF2[:, 0:4]
            for half in range(2):
                bank = bks[half]
                for f in range(NFF):
                    mm(ps(bank), actT[:, f, j * 128:(j + 1) * 128], wdb[:, f, half * 512:(half + 1) * 512], f == 0, f == NFF - 1,
                       ["actT", "wdb"], ["ps%d" % bank])
                act(otile[:, half * 512:(half + 1) * 512], ps(bank), AF.Square, ["ps%d" % bank], ["otile%d" % osl, ksq],
                    accum=sqF[:, 1 + half:2 + half])
            tt("dve", sqF[:, 1:2], sqF[:, 1:2], sqF[:, 2:3], ALU.add, [ksq], [ksq])
            act(sqF[:, 1:2], sqF[:, 1:2], AF.Sqrt, [ksq, "eps_c"], [ksq], scale=1.0 / D, bias=eps_c)
            V("dve", lambda e, sqF=sqF: e.reciprocal(out=sqF[:, 1:2], in_=sqF[:, 1:2]), [ksq], [ksq])
            for half in range(2):
                hs = slice(half * 512, (half + 1) * 512)
                stt("dve", otile[:, hs], ps(bks[half]), sqF[:, 1:2], gpost2[:, hs], ALU.mult, ALU.mult,
                    ["ps%d" % bks[half], ksq, "gpost2"], ["otile%d" % osl])
            tt("pool", otile[:, 0:512], otile[:, 0:512], x1b[:, j, 0:512], ALU.add, ["otile%d" % osl, "x1b%d" % j], ["otileA%d" % osl])
            tt("dve", otile[:, 512:1024], otile[:, 512:1024], x1b[:, j, 512:1024], ALU.add, ["otile%d" % osl, "x1b%d" % j], ["otileB%d" % osl])
            P.dma("sp", "ow%d" % osl, lambda e, r0=r0, otile=otile: e.dma_start(out=out[r0:r0 + 128, :], in_=otile),
                  reads=["otile%d" % osl, "otileA%d" % osl, "otileB%d" % osl], writes=["out%d" % osl])
    P.fence("sp", ["out0", "out1"])
    P.emit(st)
    st.close()
    return nc


INPUT_ORDER = ["x", "norm_mix_pre", "norm_mix_post", "norm_ffn_pre", "norm_ffn_post", "w_in", "ml_gate_bias",
               "s5_lam_re", "s5_lam_im", "s5_log_dt", "s5_b_re", "s5_b_im", "s5_c_re", "s5_c_im", "s5_d",
               "s5_w_glu", "s5_b_glu", "ml_conv_w", "ml_conv_b", "ml_wq", "ml_wk", "ml_wv", "ml_head_norm",
               "ml_skip", "w_out", "w_gate", "w_up", "w_down"]

_SHAPES = {"norm_mix_pre": [D], "norm_mix_post": [D], "norm_ffn_pre": [D], "norm_ffn_post": [D],
           "w_in": [D, D_IN], "ml_gate_bias": [16], "s5_lam_re": [64, 64], "s5_lam_im": [64, 64],
           "s5_log_dt": [64], "s5_b_re": [32, 64, 16], "s5_b_im": [32, 64, 16], "s5_c_re": [512, 64],
           "s5_c_im": [512, 64], "s5_d": [512], "s5_w_glu": [512, 512], "s5_b_glu": [512],
           "ml_conv_w": [5, 512], "ml_conv_b": [512], "ml_wq": [4, 128, 128], "ml_wk": [4, 128, 128],
           "ml_wv": [4, 128, 128], "ml_head_norm": [512], "ml_skip": [512], "w_out": [D, D],
           "w_gate": [D, D_FF], "w_up": [D, D_FF], "w_down": [D_FF, D]}


def make_in_map(inputs, xs):
    m = {"x": np.ascontiguousarray(xs, dtype=np.float32)}
    for k, shp in _SHAPES.items():
        m[k] = np.ascontiguousarray(np.asarray(inputs[k], dtype=np.float32).reshape(shp))
    return m


def kernel(**inputs):
    x = np.asarray(inputs["x"], dtype=np.float32)
    B, S, _ = x.shape
    ncores = 8
    nseq = B // ncores
    nc = build_program(nseq, S)
    in_maps = []
    for i in range(ncores):
        xs = x[i * nseq:(i + 1) * nseq].reshape(nseq * S, D)
        in_maps.append(make_in_map(inputs, xs))
    res = run_bass_kernel_spmd(nc, in_maps, core_ids=list(range(ncores)))
    outs = [np.asarray(r["out"], dtype=np.float32).reshape(nseq, S, D) for r in res.results]
    return np.concatenate(outs, axis=0)
```
